# Optimizing a Trainium2 kernel written in Bass

```python
import math
import jax, jax.numpy as jnp
from jax import lax
import numpy as np

D_MODEL = 1024
BATCH = 16
SEQ = 2048
DEPTH = 2

A_HEADS = 8
A_NOPE = 64
A_ROPE = 32
A_V = 64
A_Q_LORA = 256
A_KV_LORA = 128
ROPE_THETA = 10000.0
Q_BLOCK = 128
B_HEADS = 8
B_HEAD_DIM = 64
B_GROUPS = ((128, 1), (512, 4), (2048, 16))
C_HEADS = 8
C_KV_HEADS = 2
C_HEAD_DIM = 64
C_RADIUS = 128
N_BUCKETS = 32
MAX_DISTANCE = 1024
D_FF_DENSE = 2816
N_EXPERTS = 8
TOP_K = 2
D_FF_EXPERT = 3584
N_BRANCH = 3
EPS = 1e-6
NEG_INF = -1e30

A_COLS = A_Q_LORA + A_KV_LORA + A_ROPE
B_COLS = 3 * B_HEADS * B_HEAD_DIM
C_COLS = (C_HEADS + 2 * C_KV_HEADS) * C_HEAD_DIM
G_COLS = N_BRANCH * D_MODEL
IN_COLS = A_COLS + B_COLS + C_COLS + G_COLS

kernel_name = 'hybrid_gated_mla_dilated_swa_moe_encoder'


def rmsnorm(x, g):
    xf = x.astype(jnp.float32)
    y = xf * lax.rsqrt(jnp.mean(xf * xf, axis=-1, keepdims=True) + EPS)
    return (y * g.astype(jnp.float32)).astype(x.dtype)


def t5_bucket(rel):
    nb = N_BUCKETS // 2
    max_exact = nb // 2
    ret = jnp.where(rel > 0, nb, 0)
    n = jnp.abs(rel)
    nf = jnp.maximum(n, 1).astype(jnp.float32)
    large = max_exact + (jnp.log(nf / max_exact) / math.log(MAX_DISTANCE / max_exact) * (nb - max_exact)).astype(jnp.int32)
    large = jnp.minimum(large, nb - 1)
    return ret + jnp.where(n < max_exact, n, large)


def rope_tables(S):
    half = A_ROPE // 2
    inv = ROPE_THETA ** (-jnp.arange(half, dtype=jnp.float32) / half)
    ang = jnp.arange(S, dtype=jnp.float32)[:, None] * inv[None, :]
    return jnp.cos(ang), jnp.sin(ang)


def apply_rope(t, cos, sin):
    half = t.shape[-1] // 2
    cos = cos.astype(t.dtype)
    sin = sin.astype(t.dtype)
    t1, t2 = t[..., :half], t[..., half:]
    return jnp.concatenate([t1 * cos - t2 * sin, t1 * sin + t2 * cos], axis=-1)


def to_blocks(t, blk, nb):
    L = t.shape[-2]
    pad = [(0, 0)] * (t.ndim - 2) + [(0, nb * blk - L), (0, 0)]
    return jnp.pad(t, pad).reshape(t.shape[:-2] + (nb, blk, t.shape[-1]))


def to_windows(t, blk, nb):
    L = t.shape[-2]
    pad = [(0, 0)] * (t.ndim - 2) + [(blk, (nb + 1) * blk - L), (0, 0)]
    tb = jnp.pad(t, pad).reshape(t.shape[:-2] + (nb + 2, blk, t.shape[-1]))
    return jnp.concatenate([tb[..., :-2, :, :], tb[..., 1:-1, :, :], tb[..., 2:, :, :]], axis=-2)


def band_geometry(blk, nb, L):
    a = jnp.arange(blk)
    c = jnp.arange(3 * blk)
    i = jnp.arange(nb)
    rel = c[None, :] - blk - a[:, None]
    kpos = (i[:, None, None] - 1) * blk + c[None, None, :]
    valid = (jnp.abs(rel) <= blk)[None] & (kpos >= 0) & (kpos < L)
    return rel, valid


def mla_branch(cq, ckv, kpe, q_norm_g, w_uq, kv_norm_g, w_ukv):
    B, S, _ = cq.shape
    cos, sin = rope_tables(S)
    scale = (A_NOPE + A_ROPE) ** -0.5
    q = (rmsnorm(cq, q_norm_g) @ w_uq).reshape(B, S, A_HEADS, A_NOPE + A_ROPE) * scale
    q_nope = q[..., :A_NOPE]
    q_pe = apply_rope(q[..., A_NOPE:], cos[:, None, :], sin[:, None, :])
    kv = (rmsnorm(ckv, kv_norm_g) @ w_ukv).reshape(B, S, A_HEADS, A_NOPE + A_V)
    k_nope, v = kv[..., :A_NOPE], kv[..., A_NOPE:]
    k_pe = apply_rope(kpe, cos, sin)
    nb = S // Q_BLOCK
    qn_b = q_nope.reshape(B, nb, Q_BLOCK, A_HEADS, A_NOPE).transpose(1, 0, 2, 3, 4)
    qp_b = q_pe.reshape(B, nb, Q_BLOCK, A_HEADS, A_ROPE).transpose(1, 0, 2, 3, 4)

    def attend(args):
        qn, qp = args
        s = jnp.einsum('bqhd,bkhd->bhqk', qn, k_nope) + jnp.einsum('bqhd,bkd->bhqk', qp, k_pe)
        p = jax.nn.softmax(s.astype(jnp.float32), axis=-1).astype(v.dtype)
        return jnp.einsum('bhqk,bkhd->bqhd', p, v)

    o = lax.map(attend, (qn_b, qp_b))
    return o.transpose(1, 0, 2, 3, 4).reshape(B, S, A_HEADS * A_V)


def dilated_branch(q, k, v, bias_tab):
    B, H, S, dh = q.shape
    q = q * (dh ** -0.5)
    ms, ls, os_ = [], [], []
    for window, dil in B_GROUPS:
        r = window // (2 * dil)
        L = S // dil
        nb = -(-L // r)

        def strided(t):
            return t.reshape(B, H, L, dil, dh).transpose(0, 1, 3, 2, 4)

        def unstride(t):
            t = t.reshape((B, H, dil, nb * r) + t.shape[5:])[:, :, :, :L]
            t = jnp.moveaxis(t, 2, 3)
            return t.reshape((B, H, S) + t.shape[4:])

        qb = to_blocks(strided(q), r, nb)
        kw = to_windows(strided(k), r, nb)
        vw = to_windows(strided(v), r, nb)
        rel, valid = band_geometry(r, nb, L)
        bias = bias_tab[t5_bucket(rel * dil)].transpose(2, 0, 1).astype(jnp.float32)
        s = jnp.einsum('bhgnqd,bhgnkd->bhgnqk', qb, kw).astype(jnp.float32) + bias[None, :, None, None]
        s = jnp.where(valid, s, NEG_INF)
        m = jnp.max(s, axis=-1, keepdims=True)
        p = jnp.exp(s - m)
        l = jnp.sum(p, axis=-1)
        o = jnp.einsum('bhgnqk,bhgnkd->bhgnqd', p.astype(v.dtype), vw).astype(jnp.float32)
        ms.append(unstride(m[..., 0]))
        ls.append(unstride(l))
        os_.append(unstride(o))
    m_all = jnp.max(jnp.stack(ms), axis=0)
    wts = [jnp.exp(mg - m_all) for mg in ms]
    num = sum(w[..., None] * og for w, og in zip(wts, os_))
    den = sum(w * lg for w, lg in zip(wts, ls))
    out = (num / den[..., None]).astype(v.dtype)
    return out.transpose(0, 2, 1, 3).reshape(B, S, H * dh)


def window_gqa_branch(q, k, v, sink, bias_tab):
    B, G, R, S, dh = q.shape
    blk = C_RADIUS
    nb = -(-S // blk)
    qb = to_blocks(q * (dh ** -0.5), blk, nb)
    kw = to_windows(k, blk, nb)
    vw = to_windows(v, blk, nb)
    rel, valid = band_geometry(blk, nb, S)
    bias = bias_tab[t5_bucket(rel)].reshape(blk, 3 * blk, G, R).transpose(2, 3, 0, 1).astype(jnp.float32)
    s = jnp.einsum('bgrnqd,bgnkd->bgrnqk', qb, kw).astype(jnp.float32) + bias[None, :, :, None]
    s = jnp.where(valid, s, NEG_INF)
    sk = sink.astype(jnp.float32).reshape(G, R)[None, :, :, None, None, None]
    m = jnp.maximum(jnp.max(s, axis=-1, keepdims=True), sk)
    p = jnp.exp(s - m)
    den = jnp.sum(p, axis=-1) + jnp.exp(sk - m)[..., 0]
    o = jnp.einsum('bgrnqk,bgnkd->bgrnqd', p.astype(v.dtype), vw).astype(jnp.float32) / den[..., None]
    o = o.reshape(B, G, R, nb * blk, dh)[:, :, :, :S].astype(v.dtype)
    return o.transpose(0, 3, 1, 2, 4).reshape(B, S, G * R * dh)


def hybrid_mixer(h, w_in, q_norm_g, w_uq, kv_norm_g, w_ukv, sink, rel_bias, w_br_a, w_br_b, w_br_c, w_out):
    B, S, D = h.shape
    z = h @ w_in
    o = 0
    cq = z[..., o:o + A_Q_LORA]; o += A_Q_LORA
    ckv = z[..., o:o + A_KV_LORA]; o += A_KV_LORA
    kpe = z[..., o:o + A_ROPE]; o += A_ROPE
    zb = z[..., o:o + B_COLS]; o += B_COLS
    zc = z[..., o:o + C_COLS]; o += C_COLS
    zg = z[..., o:o + G_COLS]
    y_a = mla_branch(cq, ckv, kpe, q_norm_g, w_uq, kv_norm_g, w_ukv)
    qkv_b = zb.reshape(B, S, 3, B_HEADS, B_HEAD_DIM).transpose(2, 0, 3, 1, 4)
    y_b = dilated_branch(qkv_b[0], qkv_b[1], qkv_b[2], rel_bias[:, :B_HEADS])
    R = C_HEADS // C_KV_HEADS
    nq = C_HEADS * C_HEAD_DIM
    nkv = C_KV_HEADS * C_HEAD_DIM
    q_c = zc[..., :nq].reshape(B, S, C_KV_HEADS, R, C_HEAD_DIM).transpose(0, 2, 3, 1, 4)
    k_c = zc[..., nq:nq + nkv].reshape(B, S, C_KV_HEADS, C_HEAD_DIM).transpose(0, 2, 1, 3)
    v_c = zc[..., nq + nkv:].reshape(B, S, C_KV_HEADS, C_HEAD_DIM).transpose(0, 2, 1, 3)
    y_c = window_gqa_branch(q_c, k_c, v_c, sink, rel_bias[:, B_HEADS:])
    gates = jax.nn.sigmoid(zg).reshape(B, S, N_BRANCH, D)
    merged = (gates[:, :, 0] * (y_a @ w_br_a) + gates[:, :, 1] * (y_b @ w_br_b)
              + gates[:, :, 2] * (y_c @ w_br_c))
    return merged @ w_out


def swiglu(h, wg, wu, wd):
    return (jax.nn.silu(h @ wg) * (h @ wu)) @ wd


def moe_swiglu(h, w_router, wg, wu, wd):
    logits = (h @ w_router).astype(jnp.float32)
    top_v, top_i = lax.top_k(logits, TOP_K)
    gate = jax.nn.softmax(top_v, axis=-1)
    combine = jnp.sum(jax.nn.one_hot(top_i, N_EXPERTS, dtype=jnp.float32) * gate[..., None], axis=-2)
    out = jnp.zeros_like(h)
    for e in range(N_EXPERTS):
        out = out + combine[..., e:e + 1].astype(h.dtype) * swiglu(h, wg[e], wu[e], wd[e])
    return out


def setup_inputs(seed: int = 0) -> dict:
    key = jax.random.key(seed)
    ks = iter(jax.random.split(key, 32))
    n_dense = (DEPTH + 1) // 2
    n_moe = DEPTH // 2
    L = DEPTH

    def nrm(shape, scale):
        return jax.random.normal(next(ks), shape, jnp.float32) * scale

    def gain(shape):
        return 1.0 + nrm(shape, 0.05)

    return {
        'x': nrm((BATCH, SEQ, D_MODEL), 1.0),
        'norm1_g': gain((L, D_MODEL)),
        'w_in': nrm((L, D_MODEL, IN_COLS), D_MODEL ** -0.5),
        'q_norm_g': gain((L, A_Q_LORA)),
        'w_uq': nrm((L, A_Q_LORA, A_HEADS * (A_NOPE + A_ROPE)), A_Q_LORA ** -0.5),
        'kv_norm_g': gain((L, A_KV_LORA)),
        'w_ukv': nrm((L, A_KV_LORA, A_HEADS * (A_NOPE + A_V)), A_KV_LORA ** -0.5),
        'sink_logit': nrm((L, C_HEADS), 0.5),
        'rel_bias': nrm((N_BUCKETS, B_HEADS + C_HEADS), 0.5),
        'w_branch_a': nrm((L, A_HEADS * A_V, D_MODEL), (A_HEADS * A_V) ** -0.5),
        'w_branch_b': nrm((L, B_HEADS * B_HEAD_DIM, D_MODEL), (B_HEADS * B_HEAD_DIM) ** -0.5),
        'w_branch_c': nrm((L, C_HEADS * C_HEAD_DIM, D_MODEL), (C_HEADS * C_HEAD_DIM) ** -0.5),
        'w_out': nrm((L, D_MODEL, D_MODEL), D_MODEL ** -0.5),
        'norm2_g': gain((L, D_MODEL)),
        'ffn_w_gate': nrm((n_dense, D_MODEL, D_FF_DENSE), D_MODEL ** -0.5),
        'ffn_w_up': nrm((n_dense, D_MODEL, D_FF_DENSE), D_MODEL ** -0.5),
        'ffn_w_down': nrm((n_dense, D_FF_DENSE, D_MODEL), D_FF_DENSE ** -0.5),
        'router_w': nrm((n_moe, D_MODEL, N_EXPERTS), D_MODEL ** -0.5),
        'exp_w_gate': nrm((n_moe, N_EXPERTS, D_MODEL, D_FF_EXPERT), D_MODEL ** -0.5),
        'exp_w_up': nrm((n_moe, N_EXPERTS, D_MODEL, D_FF_EXPERT), D_MODEL ** -0.5),
        'exp_w_down': nrm((n_moe, N_EXPERTS, D_FF_EXPERT, D_MODEL), D_FF_EXPERT ** -0.5),
        'final_g': gain((D_MODEL,)),
    }


def reference(x, norm1_g, w_in, q_norm_g, w_uq, kv_norm_g, w_ukv, sink_logit, rel_bias,
              w_branch_a, w_branch_b, w_branch_c, w_out, norm2_g, ffn_w_gate, ffn_w_up,
              ffn_w_down, router_w, exp_w_gate, exp_w_up, exp_w_down, final_g):
    for l in range(DEPTH):
        h = rmsnorm(x, norm1_g[l])
        x = x + hybrid_mixer(h, w_in[l], q_norm_g[l], w_uq[l], kv_norm_g[l], w_ukv[l], sink_logit[l],
                             rel_bias, w_branch_a[l], w_branch_b[l], w_branch_c[l], w_out[l])
        h = rmsnorm(x, norm2_g[l])
        i = l // 2
        if l % 2 == 0:
            x = x + swiglu(h, ffn_w_gate[i], ffn_w_up[i], ffn_w_down[i])
        else:
            x = x + moe_swiglu(h, router_w[i], exp_w_gate[i], exp_w_up[i], exp_w_down[i])
    return rmsnorm(x, final_g)
```

```python
import math
from contextlib import ExitStack

import numpy as np
import concourse.bass as bass
import concourse.mybir as mybir
from concourse.bass_utils import run_bass_kernel_spmd

F32 = mybir.dt.float32
BF16 = mybir.dt.bfloat16
AF = mybir.ActivationFunctionType
ALU = mybir.AluOpType
AX = mybir.AxisListType

S = 2048
D = 1024
KC = 8
NT = 4
TW = 512
NL = 2
A_COLS = 416
B0 = 416
C0 = 1952
G0 = 2720
IN_COLS = 5792
DFF = 2816
NE = 8
DFE = 3584
GW = 3072
D0 = 1536
HW = 2945
HWP = 2946
EPS = 1e-6
NCORES = 8
NSEQ = 2

ENGS = ["pe", "act", "dve", "pool", "sp"]
SEM_ROLL = 24000
ARENA_GROUPS = ("Y", "A")
TAB_ENG = "dve"
QK_REP = 1
KR_BC = 128
EVAC = "dve"


class Slot:
    def __init__(self, prog, name):
        self.sem = prog._new_sem("s_" + name)
        self.count = 0


class Prog:
    def __init__(self, nc, stack):
        self.nc = nc
        self.stack = stack
        self.sems = []
        self.ops = {e: [] for e in ENGS}
        self.eng_sem = {}
        self.eng_cnt = {}
        for e in ENGS:
            self.eng_sem[e] = self._new_sem("e_" + e)
            self.eng_cnt[e] = 0
        self.state = {}
        self.waited = {}
        self.fence = {}
        self.nroll = 0

    def _new_sem(self, name):
        s = self.stack.enter_context(self.nc.semaphore(name))
        self.sems.append(s)
        return len(self.sems) - 1

    def _deps(self, eng, reads, writes):
        deps = []
        for k in reads:
            st = self.state.get(k)
            if st is None:
                if k[0] in ARENA_GROUPS:
                    for t in self.fence.get("ARENA", ()):
                        deps.append((t, "raw"))
            elif st[0] is not None:
                deps.append((st[0], "raw"))
        for k in writes:
            st = self.state.get(k)
            if st is None:
                if k[0] in ARENA_GROUPS:
                    for t in self.fence.get("ARENA", ()):
                        deps.append((t, "raw"))
            else:
                if st[0] is not None:
                    deps.append((st[0], "waw"))
                for r in st[1]:
                    deps.append((r, "war"))
        waits = []
        for tok, kind in deps:
            semid, val, teng, is_dma = tok
            if (not is_dma) and teng == eng:
                if eng == "pe":
                    continue
            key = (eng, semid)
            if self.waited.get(key, 0) >= val:
                continue
            self.waited[key] = val
            waits.append((semid, val))
        return waits

    def _update(self, tok, reads, writes):
        for k in reads:
            st = self.state.get(k)
            if st is None:
                st = self.state[k] = [None, []]
            st[1].append(tok)
            if len(st[1]) > 24:
                best = {}
                for t in st[1]:
                    if t[0] not in best or best[t[0]][1] < t[1]:
                        best[t[0]] = t
                st[1] = list(best.values())
        for k in writes:
            self.state[k] = [tok, []]

    def do_fence(self, groups):
        best = {}
        for t in self.fence.get("ARENA", ()):
            best[t[0]] = t
        dead = [k for k in self.state if k[0] in ARENA_GROUPS]
        for k in dead:
            st = self.state.pop(k)
            toks = list(st[1])
            if st[0] is not None:
                toks.append(st[0])
            for t in toks:
                if t[0] not in best or best[t[0]][1] < t[1]:
                    best[t[0]] = t
        self.fence["ARENA"] = list(best.values())

    def op(self, eng, fn, reads=(), writes=()):
        waits = self._deps(eng, reads, writes)
        if self.eng_cnt[eng] >= SEM_ROLL:
            self.nroll += 1
            self.eng_sem[eng] = self._new_sem("e_%s_%d" % (eng, self.nroll))
            self.eng_cnt[eng] = 0
        self.eng_cnt[eng] += 1
        tok = (self.eng_sem[eng], self.eng_cnt[eng], eng, False)
        self.ops[eng].append((fn, waits, self.eng_sem[eng], 1))
        self._update(tok, reads, writes)

    def dma(self, eng, fn, slot, reads=(), writes=()):
        waits = self._deps(eng, reads, writes)
        slot.count += 16
        tok = (slot.sem, slot.count, eng, True)
        self.ops[eng].append((fn, waits, slot.sem, 16))
        self._update(tok, reads, writes)

    def wait_all(self, eng, keys):
        waits = self._deps(eng, keys, ())
        self.ops[eng].append((None, waits, None, 0))

    def emit(self):
        nc = self.nc
        sems = self.sems
        ops = self.ops
        with nc.Block() as block:
            def run(e, lst):
                for fn, waits, semid, inc in lst:
                    for (sid, val) in waits:
                        e.wait_ge(sems[sid], val)
                    if fn is None:
                        continue
                    fn(e).then_inc(sems[semid], inc)

            @block.tensor
            def _(e):
                run(e, ops["pe"])

            @block.scalar
            def _(e):
                run(e, ops["act"])

            @block.vector
            def _(e):
                run(e, ops["dve"])

            @block.gpsimd
            def _(e):
                run(e, ops["pool"])

            @block.sync
            def _(e):
                run(e, ops["sp"])


def _t5_bucket(rel):
    nb = 16
    max_exact = 8
    ret = np.where(rel > 0, nb, 0)
    n = np.abs(rel)
    nf = np.maximum(n, 1).astype(np.float32)
    large = max_exact + (np.log(nf / np.float32(max_exact)) / np.float32(math.log(1024 / max_exact))
                         * np.float32(nb - max_exact)).astype(np.int32)
    large = np.minimum(large, nb - 1)
    return ret + np.where(n < max_exact, n, large)


def _const_tables():
    half = 16
    inv = (np.float32(10000.0) ** (-np.arange(half, dtype=np.float32) / np.float32(half))).astype(np.float32)
    ang = np.arange(S, dtype=np.float32)[:, None] * inv[None, :]
    cos = np.cos(ang).astype(np.float32).T
    sin = np.sin(ang).astype(np.float32).T
    rope = np.ones((128, S), np.float32)
    rope[64:80] = cos
    rope[80:96] = cos
    rope[96:112] = -sin
    rope[112:128] = sin
    delta = np.arange(GW, dtype=np.int64) - D0
    bk = _t5_bucket(delta.astype(np.int32))
    oh = np.zeros((32, GW), np.float32)
    oh[bk, np.arange(GW)] = 1.0
    ad = np.abs(delta)
    mb = ((ad <= 64).astype(np.float32) + ((delta % 4 == 0) & (ad <= 256)).astype(np.float32)
          + ((delta % 16 == 0) & (ad <= 1024)).astype(np.float32))
    mc = (ad <= 128).astype(np.float32)
    mult = np.zeros((16, GW), np.float32)
    mult[:8] = mb[None]
    mult[8:] = mc[None]
    ident = np.eye(128, dtype=np.float32)
    return rope, oh, mult, ident


def build(nseq=NSEQ, dbg=None, stop=99, do_tables=True, xp=0, flags=()):
    nc = bass.Bass("TRN2", target_bir_lowering=False)

    def din(name, shape, dt=F32):
        return nc.dram_tensor(name, list(shape), dt, kind="ExternalInput").ap()

    x_d = din("x", [nseq, S, D])
    w_in_d = din("w_in", [NL, D, IN_COLS])
    w_uq_d = din("w_uq", [NL, 256, 768])
    w_ukv_d = din("w_ukv", [NL, 128, 1024])
    rel_bias_d = din("rel_bias", [32, 16])
    w_br_d = [din("w_branch_a", [NL, 512, D]), din("w_branch_b", [NL, 512, D]), din("w_branch_c", [NL, 512, D])]
    w_out_d = din("w_out", [NL, D, D])
    ffn_wg_d = din("ffn_w_gate", [1, D, DFF])
    ffn_wu_d = din("ffn_w_up", [1, D, DFF])
    ffn_wd_d = din("ffn_w_down", [1, DFF, D])
    router_d = din("router_w", [1, D, NE])
    exp_wg_d = din("exp_w_gate", [1, NE, D, DFE])
    exp_wu_d = din("exp_w_up", [1, NE, D, DFE])
    exp_wd_d = din("exp_w_down", [1, NE, DFE, D])
    gains_d = din("gains", [128, 40])
    qg_d = din("qg", [128, 4])
    kvg_d = din("kvg", [128, 2])
    sinkb_d = din("sinkb", [128, 16])
    c_rope_d = din("c_rope", [128, S])
    c_oh_d = din("c_oh", [32, GW])
    c_mult_d = din("c_mult", [16, GW])
    c_ident_d = din("c_ident", [128, 128])
    out_d = nc.dram_tensor("out", [nseq, S, D], F32, kind="ExternalOutput").ap()
    gtab_d = nc.dram_tensor("gtab", [16, GW], BF16, kind="Internal").ap()
    dbg_d = None
    if dbg is not None:
        dbg_d = nc.dram_tensor("dbg", [128, 12 * S], BF16, kind="ExternalOutput").ap()

    with ExitStack() as st:
        P = Prog(nc, st)

        def sb(name, shape, dt):
            return st.enter_context(nc.sbuf_tensor(name, list(shape), dt))

        xT = sb("xT", [128, KC, S], F32)
        hT = sb("hT", [128, KC, S], BF16)
        arena = sb("arena", [128, 49152], BF16)
        wx = sb("wx", [128, 7168], BF16)
        ident = sb("ident", [128, 128], F32)
        onesb = sb("onesb", [128, 128], BF16)
        onesf = sb("onesf", [128, 128], F32)
        cst = sb("cst", [128, 8], F32)
        gains = sb("gains_sb", [128, 40], F32)
        qg = sb("qg_sb", [128, 4], F32)
        kvg = sb("kvg_sb", [128, 2], F32)
        esink = sb("esink", [128, 16], F32)
        ps = [st.enter_context(nc.psum_tensor("ps%d" % i, [128, 512], F32)) for i in range(8)]

        def cv(off, n, dt=BF16):
            if dt == BF16:
                return arena[:, off:off + n]
            return arena[:, off:off + 2 * n].bitcast(F32)

        slots = {}

        def slot(name):
            if name not in slots:
                slots[name] = Slot(P, name)
            return slots[name]

        PSK = [("PS", i) for i in range(8)]

        def mm(out, lhsT, rhs, start, stop, reads, writes):
            P.op("pe", lambda e: e.matmul(out, lhsT=lhsT, rhs=rhs, start=start, stop=stop), reads, writes)

        def tr(out, in_, reads, writes):
            P.op("pe", lambda e: e.transpose(out, in_, ident[:]), reads, writes)

        def act(out, in_, func, reads, writes, scale=None, bias=None):
            kw = {}
            if scale is not None:
                kw["scale"] = scale
            if bias is not None:
                kw["bias"] = bias
            P.op("act", lambda e: e.activation(out=out, in_=in_, func=func, **kw), reads, writes)

        def cp(eng, out, in_, reads, writes):
            if eng == "act":
                P.op("act", lambda e: e.activation(out=out, in_=in_, func=AF.Copy), reads, writes)
            else:
                P.op(eng, lambda e: e.tensor_copy(out=out, in_=in_), reads, writes)

        def tt(out, in0, in1, op, reads, writes, eng="dve"):
            P.op(eng, lambda e: e.tensor_tensor(out=out, in0=in0, in1=in1, op=op), reads, writes)

        def ts(out, in0, s1, op0, reads, writes, s2=None, op1=None, eng="dve"):
            if op1 is None:
                P.op(eng, lambda e: e.tensor_scalar(out=out, in0=in0, scalar1=s1, scalar2=None, op0=op0), reads, writes)
            else:
                P.op(eng, lambda e: e.tensor_scalar(out=out, in0=in0, scalar1=s1, scalar2=s2, op0=op0, op1=op1),
                     reads, writes)

        def stt(out, in0, scalar, in1, op0, op1, reads, writes):
            P.op("dve", lambda e: e.scalar_tensor_tensor(out=out, in0=in0, scalar=scalar, in1=in1, op0=op0, op1=op1),
                 reads, writes)

        def recip(out, in_, reads, writes):
            P.op("dve", lambda e: e.reciprocal(out=out, in_=in_), reads, writes)

        def memset(eng, ap, val, writes):
            P.op(eng, lambda e: e.memset(ap, val), (), writes)

        def dma(eng, out, in_, sl, reads, writes):
            P.dma(eng, lambda e: e.dma_start(out=out, in_=in_), sl, reads, writes)

        def fence():
            P.do_fence(["Y", "A"])

        memset("dve", onesb[:], 1.0, [("C", "onesb")])
        memset("dve", onesf[:], 1.0, [("C", "onesf")])
        memset("dve", cst[:, 0:1], EPS, [("C", "cst")])
        dma("sp", ident[:], c_ident_d, slot("ident"), [], [("C", "ident")])
        dma("sp", gains[:], gains_d, slot("gains"), [], [("C", "gains")])
        dma("sp", qg[:], qg_d, slot("qg"), [], [("C", "qg")])
        dma("sp", kvg[:], kvg_d, slot("kvg"), [], [("C", "kvg")])
        dma("sp", esink[:], sinkb_d, slot("esink"), [], [("C", "esink0")])
        act(esink[:], esink[:], AF.Exp, [("C", "esink0")], [("C", "esink")])

        oh_sb = cv(0, 3072, F32)
        mult_sb = cv(6144, 3072, F32)
        gexp_sb = cv(12288, 3072, F32)
        gw_sb = cv(18432, 3072, BF16)
        rb_sb = cv(22016, 16, F32)
        dma("sp", oh_sb[0:32, :], c_oh_d, slot("oh"), [], [("A", "oh")])
        dma("sp", mult_sb[0:16, :], c_mult_d, slot("mult"), [], [("A", "mult")])
        dma("sp", rb_sb[0:32, :], rel_bias_d, slot("rb"), [], [("A", "rb")])
        for j in range(GW // 512):
            mm(ps[6][0:16, :], rb_sb[0:32, :], oh_sb[0:32, j * 512:(j + 1) * 512], True, True,
               [("A", "oh"), ("A", "rb")], [PSK[6]])
            act(gexp_sb[0:16, j * 512:(j + 1) * 512], ps[6][0:16, :], AF.Exp, [PSK[6]], [("A", "gexp", j)])
            tt(gw_sb[0:16, j * 512:(j + 1) * 512], gexp_sb[0:16, j * 512:(j + 1) * 512],
               mult_sb[0:16, j * 512:(j + 1) * 512], ALU.mult, [("A", "gexp", j), ("A", "mult")], [("A", "gw", j)])
        dma("sp", gtab_d, gw_sb[0:16, :], slot("gtab"), [("A", "gw", j) for j in range(GW // 512)], [("D", "gtab")])
        fence()

        yT = arena[:, 0:24576].rearrange("p (c s) -> p c s", c=12)
        U = 24576
        Qb = [cv(U + i * 2048, 2048) for i in range(2)]
        Kb = [cv(U + 4096 + i * 2048, 2048) for i in range(2)]
        Vb = [cv(U + 8192 + i * 2048, 2048).rearrange("p (b c) -> p b c", b=16) for i in range(2)]
        Pt = [cv(U + 12288 + i * 512, 512) for i in range(3)] + [cv(U + 14848, 512)]
        SB = [0, 1, 2, 7]
        rd = cv(U + 13824, 512, F32)
        dtmp = cv(U + 14848, 512, F32)
        R = U + 15872
        TOP = 40960
        sq = cv(TOP, 4096).rearrange("p (k t) -> p k t", k=8)
        sd = cv(TOP + 4096, 512, F32)
        rstd = cv(TOP + 5120, 512, F32)
        RT = TOP + 6144

        def rmsnorm_tile(t, gcol, out_fn, out_keys_fn, nbank):
            tsl = slice(t * TW, (t + 1) * TW)
            xk = [("X", kc, t) for kc in range(KC)]
            act(sq, xT[:, :, tsl], AF.Square, xk, [("A", "sq")])
            for kc in range(KC):
                mm(ps[nbank][:, :], onesb[:], sq[:, kc, :], kc == 0, kc == KC - 1,
                   [("A", "sq"), ("C", "onesb")], [PSK[nbank]])
            act(sd, ps[nbank][:, :], AF.Sqrt, [PSK[nbank], ("C", "cst")], [("A", "sd")], scale=1.0 / D, bias=cst[:, 0:1])
            recip(rstd, sd, [("A", "sd")], [("A", "rstd")])
            for kc in range(KC):
                stt(out_fn(kc), xT[:, kc, tsl], gains[:, gcol + kc:gcol + kc + 1], rstd, ALU.mult, ALU.mult,
                    [("X", kc, t), ("A", "rstd"), ("C", "gains")], out_keys_fn(kc))

        def norm_to_h(gcol):
            for t in range(NT):
                tsl = slice(t * TW, (t + 1) * TW)
                rmsnorm_tile(t, gcol, lambda kc: hT[:, kc, tsl], lambda kc: [("H", kc, t)], 2 + (t % 2))

        att_q = []
        att_n = [0]

        def att_drain():
            while att_q:
                att_q.pop(0)()

        def attention(Q, K, V, Kr, lo, hi, table, tkey, scale, sinkcol, h, qkeys, kkeys, vkey, ychunk0, fillers=()):
            parity = h % 2
            nr = slice(64, 128) if parity else slice(0, 64)
            dr = slice(0, 64) if parity else slice(64, 128)
            chunk = ychunk0 + h // 2
            DEPTH = 3
            fillers = list(fillers)
            nfill = len(fillers)
            steps = []
            for qt in range(NT):
                kbs = [kb for kb in range(16) if lo <= kb - 4 * qt <= hi]
                for i, kb in enumerate(kbs):
                    steps.append((qt, kb, i == 0, i == len(kbs) - 1))
            total_steps = len(steps)
            queue = att_q
            fdone = [0]

            def finalize(qt):
                qsl = slice(qt * TW, (qt + 1) * TW)
                ob = 3 + (qt % 2)
                if sinkcol is not None:
                    act(rd[nr, :], ps[ob][dr, :], AF.Ln, [PSK[ob], ("C", "esink")], [("A", "rd")],
                        bias=esink[dr, sinkcol:sinkcol + 1])
                else:
                    act(rd[nr, :], ps[ob][dr, :], AF.Ln, [PSK[ob]], [("A", "rd")])
                act(rd[nr, :], rd[nr, :], AF.Exp, [("A", "rd")], [("A", "rd")], scale=-1.0)
                tt(yT[nr, chunk, qsl], ps[ob][nr, :], rd[nr, :], ALU.mult, [PSK[ob], ("A", "rd")],
                   [("Y", chunk, qt, parity)])

            def make_pv(pb_, pkb, pqt, first, last):
                def f():
                    ob = 3 + (pqt % 2)
                    mm(ps[ob][:, :], V[:, pkb, :], Pt[pb_], first, last, [("A", "Pt", pb_), vkey], [PSK[ob]])
                    if last:
                        finalize(pqt)
                return f

            for si, (qt, kb, first, last) in enumerate(steps):
                qsl = slice(qt * TW, (qt + 1) * TW)
                b = att_n[0] % 4
                att_n[0] += 1
                sb_ = SB[b]
                mm(ps[sb_][:, :], K[0:Kr, kb * 128:(kb + 1) * 128], Q[0:Kr, qsl], True, True,
                   _flat([qkeys[qt], kkeys[kb // 4]]), [PSK[sb_]])
                act(Pt[b], ps[sb_][:, :], AF.Exp, [PSK[sb_]], [("A", "Pt", b)], scale=scale)
                if table is not None:
                    u0 = D0 + 128 * (kb - 4 * qt)
                    tt(Pt[b], Pt[b], table[:, u0:u0 - 512:-1], ALU.mult, [("A", "Pt", b), tkey], [("A", "Pt", b)],
                       eng=TAB_ENG)
                queue.append(make_pv(b, kb, qt, first, last))
                if len(queue) > DEPTH:
                    queue.pop(0)()
                while fdone[0] < nfill and fdone[0] * total_steps < (si + 1) * nfill:
                    fillers[fdone[0]]()
                    fdone[0] += 1
            while fdone[0] < nfill:
                fillers[fdone[0]]()
                fdone[0] += 1

        def mixer_a(l):
            rope = arena[:, 8192:12288].bitcast(F32)
            qnT = arena[:, 12288:16384].rearrange("p (c s) -> p c s", c=2)
            kvnT = arena[:, 16384:18432]
            sqA = arena[:, 18432:19968].rearrange("p (c t) -> p c t", c=3)
            sdA = arena[:, 19968:22016].bitcast(F32).rearrange("p (c t) -> p c t", c=2)
            kt1 = arena[:, 22016:23040].bitcast(F32)
            kt2 = arena[:, 23040:24064].bitcast(F32)
            Wq = cv(R, 2048).rearrange("p (k h c) -> p k h c", k=2, h=8)
            Wkv = cv(R + 2048, 1024)
            Wa = cv(R + 3072, 3072).rearrange("p (k c) -> p k c", k=8)
            Wkpe = cv(R + 6144, 1024).rearrange("p (k c) -> p k c", k=8)
            win = w_in_d[l].rearrange("(k p) c -> p k c", p=128)
            memset("pool", Vb[0][:, :, 64:128], 1.0, [("A", "V", 0)])
            memset("pool", Vb[1][:, :, 0:64], 1.0, [("A", "V", 1)])
            dma("sp", rope, c_rope_d, slot("rope"), [], [("Y", "rope")])
            dma("pool", Wa, win[:, :, 0:384], slot("Wa"), [], [("A", "Wa")])
            dma("pool", Wkpe[:, :, 0:96], win[:, :, 320:416], slot("Wkpe"), [], [("A", "Wkpe")])
            dma("pool", Wkpe[:, :, 96:112], win[:, :, 400:416], slot("Wkpe"), [], [("A", "Wkpe")])
            dma("pool", Wkpe[:, :, 112:128], win[:, :, 384:400], slot("Wkpe"), [], [("A", "Wkpe")])
            wuq = w_uq_d[l].rearrange("(k p) (h c) -> p k h c", p=128, h=8)
            for kc in range(2):
                dma("pool", Wq[:, kc, :, 0:96], wuq[:, kc, :, :], slot("Wq"), [], [("A", "Wq")])
                dma("pool", Wq[:, kc, :, 96:112], wuq[:, kc, :, 80:96], slot("Wq"), [], [("A", "Wq")])
                dma("pool", Wq[:, kc, :, 112:128], wuq[:, kc, :, 64:80], slot("Wq"), [], [("A", "Wq")])
            dma("pool", Wkv, w_ukv_d[l], slot("Wkv"), [], [("A", "Wkv")])
            for t in range(NT):
                tsl = slice(t * TW, (t + 1) * TW)
                hk = [("H", kc, t) for kc in range(KC)]
                for c in range(3):
                    for kc in range(KC):
                        mm(ps[c][:, :], Wa[:, kc, c * 128:(c + 1) * 128], hT[:, kc, tsl], kc == 0, kc == KC - 1,
                           [("A", "Wa"), hk[kc]], [PSK[c]])
                for kc in range(KC):
                    mm(ps[3][:, :], Wkpe[:, kc, :], hT[:, kc, tsl], kc == 0, kc == KC - 1, [("A", "Wkpe"), hk[kc]], [PSK[3]])
                for c in range(3):
                    act(sqA[:, c, :], ps[c][:, :], AF.Square, [PSK[c]], [("Y", "sqA", c)])
                for c in range(2):
                    mm(ps[4][:, :], onesb[:], sqA[:, c, :], c == 0, c == 1, [("Y", "sqA", c), ("C", "onesb")], [PSK[4]])
                mm(ps[5][:, :], onesb[:], sqA[:, 2, :], True, True, [("Y", "sqA", 2), ("C", "onesb")], [PSK[5]])
                act(sdA[:, 0, :], ps[4][:, :], AF.Sqrt, [PSK[4], ("C", "cst")], [("Y", "sdA", 0)], scale=1.0 / 256, bias=cst[:, 0:1])
                act(sdA[:, 1, :], ps[5][:, :], AF.Sqrt, [PSK[5], ("C", "cst")], [("Y", "sdA", 1)], scale=1.0 / 128, bias=cst[:, 0:1])
                recip(sdA[:, 0, :], sdA[:, 0, :], [("Y", "sdA", 0)], [("Y", "sdA", 0)])
                recip(sdA[:, 1, :], sdA[:, 1, :], [("Y", "sdA", 1)], [("Y", "sdA", 1)])
                for c in range(2):
                    stt(qnT[:, c, tsl], ps[c][:, :], qg[:, 2 * l + c:2 * l + c + 1], sdA[:, 0, :], ALU.mult, ALU.mult,
                        [PSK[c], ("Y", "sdA", 0), ("C", "qg")], [("Y", "qnT", c, t)])
                stt(kvnT[:, tsl], ps[2][:, :], kvg[:, l:l + 1], sdA[:, 1, :], ALU.mult, ALU.mult,
                    [PSK[2], ("Y", "sdA", 1), ("C", "kvg")], [("Y", "kvnT", t)])
                tt(kt1[64:128, :], ps[3][64:128, :], rope[64:128, tsl], ALU.mult, [PSK[3], ("Y", "rope")], [("Y", "kt1")])
                cp("act", kt2[64:96, :], kt1[96:128, :], [("Y", "kt1")], [("Y", "kt2")])
                tt(Kb[0][64:96, tsl], kt1[64:96, :], kt2[64:96, :], ALU.add, [("Y", "kt1"), ("Y", "kt2")], [("A", "Kpe", 0, t, 0)])
                cp("act", Kb[0][96:128, tsl], Kb[0][64:96, tsl], [("A", "Kpe", 0, t, 0)], [("A", "Kpe", 0, t, 1)])
                cp("act", Kb[1][64:96, tsl], Kb[0][64:96, tsl], [("A", "Kpe", 0, t, 0)], [("A", "Kpe", 1, t, 0)])
                cp("act", Kb[1][96:128, tsl], Kb[0][64:96, tsl], [("A", "Kpe", 0, t, 0)], [("A", "Kpe", 1, t, 1)])

            def proj_chunks(h):
                hb = h % 2
                voff = 64 if hb else 0
                chunks = []

                def mk_t(t):
                    def f():
                        tsl = slice(t * TW, (t + 1) * TW)
                        for kc in range(2):
                            mm(ps[5][:, :], Wq[:, kc, h, :], qnT[:, kc, tsl], kc == 0, kc == 1,
                               [("A", "Wq"), ("Y", "qnT", kc, t)], [PSK[5]])
                        tt(Qb[hb][:, tsl], ps[5][:, :], rope[:, tsl], ALU.mult, [PSK[5], ("Y", "rope")], [("A", "Q", hb, t)])
                        mm(ps[5][0:64, :], Wkv[:, h * 128:h * 128 + 64], kvnT[:, tsl], True, True,
                           [("A", "Wkv"), ("Y", "kvnT", t)], [PSK[5]])
                        cp(EVAC, Kb[hb][0:64, tsl], ps[5][0:64, :], [PSK[5]], [("A", "K", hb, t)])
                    return f

                def mk_v(half):
                    def f():
                        bank = 6
                        pv = ps[bank][:, :].rearrange("p (b c) -> p b c", b=8)
                        for j in range(8):
                            tb = half * 8 + j
                            mm(pv[:, j, :], kvnT[:, tb * 128:(tb + 1) * 128], Wkv[:, h * 128 + 64:h * 128 + 128], True, True,
                               [("A", "Wkv"), ("Y", "kvnT", tb // 4)], [PSK[bank]])
                        cp(EVAC, Vb[hb][:, half * 8:half * 8 + 8, voff:voff + 64], pv, [PSK[bank]], [("A", "V", hb)])
                    return f

                for t in range(NT):
                    chunks.append(mk_t(t))
                for half in range(2):
                    chunks.append(mk_v(half))
                return chunks

            for c in proj_chunks(0):
                c()
            for h in range(8):
                hb = h % 2
                attention(Qb[hb], Kb[hb], Vb[hb], 128, -100, 100, None, None, 96.0 ** -0.5, None, h,
                          [("A", "Q", hb, t) for t in range(NT)],
                          [KeyList([("A", "K", hb, t), ("A", "Kpe", hb, t, 0), ("A", "Kpe", hb, t, 1)]) for t in range(NT)],
                          ("A", "V", hb), 0, fillers=proj_chunks(h + 1) if h + 1 < 8 else ())
            att_drain()

        def load_table(hidx, buf, tb_ap):
            if xp == 1:
                return
            src = bass.AP(gtab_d.tensor, hidx * GW, [[1, 128], [1, HW]])
            dma("sp", tb_ap, src, slot("tab%d" % buf), [("D", "gtab")], [("A", "tab", buf)])

        def mixer_b(l):
            Wb = cv(R, 1536).rearrange("p (k c) -> p k c", k=8)
            tabs = [cv(R + 1536 + i * HWP, HW) for i in range(2)]
            win = w_in_d[l].rearrange("(k p) c -> p k c", p=128)

            def proj_chunks(h):
                hb = h % 2
                voff = 64 if hb else 0
                chunks = []

                def c_load():
                    for w in range(3):
                        c0 = B0 + w * 512 + h * 64
                        dma("pool", Wb[:, :, w * 64:(w + 1) * 64], win[:, :, c0:c0 + 64], slot("Wb"), [], [("A", "Wb")])
                    load_table(h, hb, tabs[hb])
                chunks.append(c_load)

                def mk_qk(t, w, dst, nm):
                    def f():
                        tsl = slice(t * TW, (t + 1) * TW)
                        for kc in range(KC):
                            mm(ps[5][0:64, :], Wb[:, kc, w * 64:(w + 1) * 64], hT[:, kc, tsl], kc == 0, kc == KC - 1,
                               [("A", "Wb"), ("H", kc, t)], [PSK[5]])
                        cp(EVAC, dst[hb][0:64, tsl], ps[5][0:64, :], [PSK[5]], [("A", nm, hb, t)])
                    return f

                def mk_v(half, j):
                    def f():
                        bank = 6
                        pv = ps[bank][:, :].rearrange("p (b c) -> p b c", b=8)
                        tb = half * 8 + j
                        for kc in range(KC):
                            mm(pv[:, j, :], hT[:, kc, tb * 128:(tb + 1) * 128], Wb[:, kc, 128:192], kc == 0, kc == KC - 1,
                               [("A", "Wb"), ("H", kc, tb // 4)], [PSK[bank]])
                        if j == 7:
                            cp(EVAC, Vb[hb][:, half * 8:half * 8 + 8, voff:voff + 64], pv, [PSK[bank]], [("A", "V", hb)])
                    return f

                for t in range(NT):
                    chunks.append(mk_qk(t, 0, Qb, "Q"))
                    chunks.append(mk_qk(t, 1, Kb, "K"))
                for half in range(2):
                    for j in range(8):
                        chunks.append(mk_v(half, j))
                return chunks

            for i in range(2):
                memset("pool", Qb[i][64:128, :], 0.0, [("A", "Qz", i)])
                memset("pool", Kb[i][64:128, :], 0.0, [("A", "Kz", i)])
            for c in proj_chunks(0):
                c()
            for h in range(8):
                hb = h % 2
                attention(Qb[hb], Kb[hb], Vb[hb], KR_BC, -8, 11, tabs[hb] if xp in (0, 3) else None, ("A", "tab", hb), 0.125, None, h,
                          [KeyList([("A", "Q", hb, t), ("A", "Qz", hb)]) for t in range(NT)],
                          [KeyList([("A", "K", hb, t), ("A", "Kz", hb)]) for t in range(NT)],
                          ("A", "V", hb), 4, fillers=proj_chunks(h + 1) if h + 1 < 8 else ())
            att_drain()

        def mixer_c(l):
            Wcq = cv(R, 512).rearrange("p (k c) -> p k c", k=8)
            Wckv = cv(R + 512, 1024).rearrange("p (k c) -> p k c", k=8)
            tabs = [cv(R + 1536 + i * HWP, HW) for i in range(2)]
            win = w_in_d[l].rearrange("(k p) c -> p k c", p=128)

            def proj_kv(g):
                ck = C0 + 512 + g * 64
                cvv = C0 + 640 + g * 64
                dma("pool", Wckv[:, :, 0:64], win[:, :, ck:ck + 64], slot("Wckv"), [], [("A", "Wckv")])
                dma("pool", Wckv[:, :, 64:128], win[:, :, cvv:cvv + 64], slot("Wckv"), [], [("A", "Wckv")])
                for t in range(NT):
                    tsl = slice(t * TW, (t + 1) * TW)
                    for kc in range(KC):
                        mm(ps[5][0:64, :], Wckv[:, kc, 0:64], hT[:, kc, tsl], kc == 0, kc == KC - 1,
                           [("A", "Wckv"), ("H", kc, t)], [PSK[5]])
                    cp(EVAC, Kb[g][0:64, tsl], ps[5][0:64, :], [PSK[5]], [("A", "K", g, t)])
                for half in range(2):
                    bank = 6
                    pv = ps[bank][:, :].rearrange("p (b c) -> p b c", b=8)
                    for j in range(8):
                        tb = half * 8 + j
                        for kc in range(KC):
                            mm(pv[:, j, :], hT[:, kc, tb * 128:(tb + 1) * 128], Wckv[:, kc, 64:128], kc == 0, kc == KC - 1,
                               [("A", "Wckv"), ("H", kc, tb // 4)], [PSK[bank]])
                    cp(EVAC, Vb[0][:, half * 8:half * 8 + 8, 0:64], pv, [PSK[bank]], [("A", "V", 0)])
                    cp("pool", Vb[1][:, half * 8:half * 8 + 8, 64:128], Vb[0][:, half * 8:half * 8 + 8, 0:64],
                       [("A", "V", 0)], [("A", "V", 1)])

            def projq_chunks(h):
                hb = h % 2
                cq = C0 + h * 64
                chunks = []

                def c_load():
                    dma("pool", Wcq, win[:, :, cq:cq + 64], slot("Wcq"), [], [("A", "Wcq")])
                    load_table(8 + h, hb, tabs[hb])
                chunks.append(c_load)

                def mk_t(t):
                    def f():
                        tsl = slice(t * TW, (t + 1) * TW)
                        for kc in range(KC):
                            mm(ps[5][0:64, :], Wcq[:, kc, :], hT[:, kc, tsl], kc == 0, kc == KC - 1,
                               [("A", "Wcq"), ("H", kc, t)], [PSK[5]])
                        cp(EVAC, Qb[hb][0:64, tsl], ps[5][0:64, :], [PSK[5]], [("A", "Q", hb, t)])
                    return f

                for t in range(NT):
                    chunks.append(mk_t(t))
                return chunks

            for g in range(2):
                att_drain()
                proj_kv(g)
                for c in projq_chunks(4 * g):
                    c()
                for r in range(4):
                    h = 4 * g + r
                    hb = h % 2
                    attention(Qb[hb], Kb[g], Vb[hb], KR_BC, -1, 4, tabs[hb], ("A", "tab", hb), 0.125, l * 8 + h, h,
                              [KeyList([("A", "Q", hb, t), ("A", "Qz", hb)]) for t in range(NT)],
                              [KeyList([("A", "K", g, t), ("A", "Kz", g)]) for t in range(NT)],
                              ("A", "V", hb), 8, fillers=projq_chunks(h + 1) if r + 1 < 4 else ())
            att_drain()

        def gate_phase(l):
            merged = cv(U, 16384).rearrange("p (c s) -> p c s", c=8)
            mtemp = cv(U + 16384, 4096, F32).rearrange("p (c s) -> p c s", c=2)
            Wg = [wx[:, i * 2048:(i + 1) * 2048].rearrange("p (k c) -> p k c", k=8) for i in range(2)]
            Wbr = [wx[:, 4096 + i * 1024:4096 + (i + 1) * 1024].rearrange("p (k c) -> p k c", k=4) for i in range(2)]
            gsb = [wx[:, 6144 + i * 512:6144 + (i + 1) * 512] for i in range(2)]
            win = w_in_d[l].rearrange("(k p) c -> p k c", p=128)
            n = 0
            m = 0
            for cp in range(4):
                for b in range(3):
                    wi = n % 2
                    n += 1
                    col = G0 + b * 1024 + cp * 256
                    dma("pool", Wg[wi], win[:, :, col:col + 256], slot("Wg%d" % wi), [], [("W", "Wg", wi)])
                    wbr = w_br_d[b][l].rearrange("(k p) c -> p k c", p=128)
                    dma("pool", Wbr[wi], wbr[:, :, cp * 256:(cp + 1) * 256], slot("Wbr%d" % wi), [], [("W", "Wbr", wi)])
                    for cc in range(2):
                        c = 2 * cp + cc
                        csl = slice(cc * 128, (cc + 1) * 128)
                        for t in range(NT):
                            tsl = slice(t * TW, (t + 1) * TW)
                            gb = m % 2
                            m += 1
                            pg = ps[gb]
                            pb = ps[2 + gb]
                            for kc in range(KC):
                                mm(pg[:, :], Wg[wi][:, kc, csl], hT[:, kc, tsl], kc == 0, kc == KC - 1,
                                   [("W", "Wg", wi), ("H", kc, t)], [PSK[gb]])
                            act(gsb[gb], pg[:, :], AF.Sigmoid, [PSK[gb]], [("W", "gsb", gb)])
                            for kc in range(4):
                                mm(pb[:, :], Wbr[wi][:, kc, csl], yT[:, b * 4 + kc, tsl], kc == 0, kc == 3,
                                   [("W", "Wbr", wi), ("Y", b * 4 + kc, t, 0), ("Y", b * 4 + kc, t, 1)], [PSK[2 + gb]])
                            if b == 0:
                                tt(mtemp[:, cc, tsl], pb[:, :], gsb[gb], ALU.mult, [PSK[2 + gb], ("W", "gsb", gb)],
                                   [("A", "mtemp", cc, t)])
                            else:
                                tt(pb[:, :], pb[:, :], gsb[gb], ALU.mult, [PSK[2 + gb], ("W", "gsb", gb)], [PSK[2 + gb]])
                                if b == 1:
                                    tt(mtemp[:, cc, tsl], pb[:, :], mtemp[:, cc, tsl], ALU.add,
                                       [("A", "mtemp", cc, t), PSK[2 + gb]], [("A", "mtemp", cc, t)])
                                else:
                                    tt(merged[:, c, tsl], pb[:, :], mtemp[:, cc, tsl], ALU.add,
                                       [("A", "mtemp", cc, t), PSK[2 + gb]], [("A", "merged", c, t)])
            wo = w_out_d[l].rearrange("(k p) c -> p k c", p=128)
            for dcp in range(4):
                wi = n % 2
                n += 1
                dma("pool", Wg[wi], wo[:, :, dcp * 256:(dcp + 1) * 256], slot("Wg%d" % wi), [], [("W", "Wg", wi)])
                for cc in range(2):
                    dc = 2 * dcp + cc
                    csl = slice(cc * 128, (cc + 1) * 128)
                    for t in range(NT):
                        tsl = slice(t * TW, (t + 1) * TW)
                        ob = 4 + (t % 2)
                        for kc in range(KC):
                            mm(ps[ob][:, :], Wg[wi][:, kc, csl], merged[:, kc, tsl], kc == 0, kc == KC - 1,
                               [("W", "Wg", wi), ("A", "merged", kc, t)], [PSK[ob]])
                        tt(xT[:, dc, tsl], ps[ob][:, :], xT[:, dc, tsl], ALU.add, [PSK[ob], ("X", dc, t)], [("X", dc, t)])

        def ffn_groups(wg_ap, wu_ap, wd_ap, dff, cwb, gcount):
            WG = [cv(i * 4096, 4096).rearrange("p (k c) -> p k c", k=8) for i in range(2)]
            WU = [cv(8192 + i * 4096, 4096).rearrange("p (k c) -> p k c", k=8) for i in range(2)]
            WD = [cv(16384 + i * 4096, 4096).rearrange("p (j c) -> p j c", j=4) for i in range(2)]
            actb = cv(24576, 8192).rearrange("p (j s) -> p j s", j=4)
            sgt = [cv(32768 + i * 512, 512) for i in range(2)]
            tmpm = [cv(33792 + i * 512, 512) for i in range(2)]
            wgv = wg_ap.rearrange("(k p) c -> p k c", p=128)
            wuv = wu_ap.rearrange("(k p) c -> p k c", p=128)
            ngroups = (dff + 511) // 512
            for gi in range(ngroups):
                wi = gcount[0] % 2
                gcount[0] += 1
                c0 = gi * 512
                gw_ = min(512, dff - c0)
                nj = gw_ // 128
                dma("pool", WG[wi][:, :, 0:gw_], wgv[:, :, c0:c0 + gw_], slot("WG%d" % wi), [], [("A", "WG", wi)])
                dma("pool", WU[wi][:, :, 0:gw_], wuv[:, :, c0:c0 + gw_], slot("WU%d" % wi), [], [("A", "WU", wi)])
                wdv = wd_ap[c0:c0 + gw_, :].rearrange("(j p) c -> p j c", p=128)
                dma("pool", WD[wi][:, 0:nj, :], wdv, slot("WD%d" % wi), [], [("A", "WD", wi)])
                n = 0
                for t in range(NT):
                    tsl = slice(t * TW, (t + 1) * TW)
                    for j in range(nj):
                        gb = n % 2
                        n += 1
                        for kc in range(KC):
                            mm(ps[gb][:, :], WG[wi][:, kc, j * 128:(j + 1) * 128], hT[:, kc, tsl], kc == 0, kc == KC - 1,
                               [("A", "WG", wi), ("H", kc, t)], [PSK[gb]])
                        for kc in range(KC):
                            mm(ps[2 + gb][:, :], WU[wi][:, kc, j * 128:(j + 1) * 128], hT[:, kc, tsl], kc == 0, kc == KC - 1,
                               [("A", "WU", wi), ("H", kc, t)], [PSK[2 + gb]])
                        act(sgt[gb], ps[gb][:, :], AF.Silu, [PSK[gb]], [("A", "sgt", gb)])
                        if cwb is None:
                            tt(actb[:, j, tsl], ps[2 + gb][:, :], sgt[gb], ALU.mult, [PSK[2 + gb], ("A", "sgt", gb)],
                               [("A", "act", j, t)])
                        else:
                            tt(tmpm[gb], ps[2 + gb][:, :], sgt[gb], ALU.mult, [PSK[2 + gb], ("A", "sgt", gb)],
                               [("A", "tmpm", gb)])
                            tt(actb[:, j, tsl], tmpm[gb], cwb[0][:, tsl], ALU.mult, [("A", "tmpm", gb), cwb[1]],
                               [("A", "act", j, t)])
                for t in range(NT):
                    tsl = slice(t * TW, (t + 1) * TW)
                    for dc in range(8):
                        ob = 4 + (dc % 4)
                        for j in range(nj):
                            mm(ps[ob][:, :], WD[wi][:, j, dc * 128:(dc + 1) * 128], actb[:, j, tsl], j == 0, j == nj - 1,
                               [("A", "WD", wi), ("A", "act", j, t)], [PSK[ob]])
                        tt(xT[:, dc, tsl], ps[ob][:, :], xT[:, dc, tsl], ALU.add, [PSK[ob], ("X", dc, t)], [("X", dc, t)])

        def ffn_dense(l):
            norm_to_h(l * 16 + 8)
            fence()
            ffn_groups(ffn_wg_d[0], ffn_wu_d[0], ffn_wd_d[0], DFF, None, [0])

        def ffn_moe(l):
            h2f = arena[:, 16384:24576].bitcast(F32).rearrange("p (k t) -> p k t", k=8)
            wr = arena[:, 32768:32768 + 128].bitcast(F32).rearrange("p (k e) -> p k e", k=8)
            logits = arena[:, RT:RT + 256].bitcast(F32).rearrange("p (b e) -> p b e", b=16)
            l2 = arena[:, RT + 256:RT + 512].bitcast(F32).rearrange("p (b e) -> p b e", b=16)
            eq1 = arena[:, RT + 512:RT + 768].bitcast(F32).rearrange("p (b e) -> p b e", b=16)
            eq2 = arena[:, RT + 768:RT + 1024].bitcast(F32).rearrange("p (b e) -> p b e", b=16)
            comb = arena[:, RT + 1024:RT + 1280].bitcast(F32).rearrange("p (b e) -> p b e", b=16)
            sm = arena[:, RT + 1280:RT + 1280 + 32 * 8].bitcast(F32).rearrange("p (v b) -> p v b", v=8)
            m1, m2, dd, ee, g1, g2 = [sm[:, i, :] for i in range(6)]
            dma("sp", wr, router_d[0].rearrange("(k p) e -> p k e", p=128), slot("wr"), [], [("A", "wr")])
            for t in range(NT):
                tsl = slice(t * TW, (t + 1) * TW)
                rmsnorm_tile(t, l * 16 + 8, lambda kc: h2f[:, kc, :], lambda kc: [("A", "h2f", kc)], 2 + (t % 2))
                for kc in range(KC):
                    cp("act", hT[:, kc, tsl], h2f[:, kc, :], [("A", "h2f", kc)], [("H", kc, t)])
                for j in range(4):
                    tb = t * 4 + j
                    for kc in range(KC):
                        mm(ps[6][:, tb * 8:tb * 8 + 8], h2f[:, kc, j * 128:(j + 1) * 128], wr[:, kc, :], kc == 0, kc == KC - 1,
                           [("A", "h2f", kc), ("A", "wr")], [PSK[6]])
            pl = ps[6][:, 0:128].rearrange("p (b e) -> p b e", b=16)
            cp("dve", logits, pl, [PSK[6]], [("A", "logits")])
            P.op("dve", lambda e: e.tensor_reduce(out=m1, in_=logits, axis=AX.X, op=ALU.max), [("A", "logits")], [("A", "m1")])
            tt(eq1, logits, m1.unsqueeze(2).broadcast_to([128, 16, 8]), ALU.is_equal, [("A", "logits"), ("A", "m1")], [("A", "eq1")])
            stt(l2, eq1, -1e30, logits, ALU.mult, ALU.add, [("A", "eq1"), ("A", "logits")], [("A", "l2")])
            P.op("dve", lambda e: e.tensor_reduce(out=m2, in_=l2, axis=AX.X, op=ALU.max), [("A", "l2")], [("A", "m2")])
            tt(eq2, l2, m2.unsqueeze(2).broadcast_to([128, 16, 8]), ALU.is_equal, [("A", "l2"), ("A", "m2")], [("A", "eq2")])
            tt(dd, m2, m1, ALU.subtract, [("A", "m1"), ("A", "m2")], [("A", "dd")])
            act(ee, dd, AF.Exp, [("A", "dd")], [("A", "ee")])
            ts(dd, ee, 1.0, ALU.add, [("A", "ee")], [("A", "dd")])
            recip(g1, dd, [("A", "dd")], [("A", "g1")])
            tt(g2, ee, g1, ALU.mult, [("A", "ee"), ("A", "g1")], [("A", "g2")])
            tt(eq1, eq1, g1.unsqueeze(2).broadcast_to([128, 16, 8]), ALU.mult, [("A", "eq1"), ("A", "g1")], [("A", "eq1")])
            tt(eq2, eq2, g2.unsqueeze(2).broadcast_to([128, 16, 8]), ALU.mult, [("A", "eq2"), ("A", "g2")], [("A", "eq2")])
            tt(comb, eq1, eq2, ALU.add, [("A", "eq1"), ("A", "eq2")], [("C", "comb")])
            fence()
            cwbs = [cv(34816 + i * 2048, 2048) for i in range(2)]
            dg = [arena[:, 38912 + i * 256:38912 + (i + 1) * 256].bitcast(F32) for i in range(2)]
            gcount = [0]
            nd = 0
            for e_ in range(NE):
                ci = e_ % 2
                for tb in range(16):
                    di = nd % 2
                    nd += 1
                    ts(dg[di], ident[:], comb[:, tb, e_:e_ + 1], ALU.mult, [("C", "ident"), ("C", "comb")], [("A", "dg", di)])
                    mm(ps[7][:, (tb % 4) * 128:(tb % 4 + 1) * 128], onesf[:], dg[di], True, True,
                       [("C", "onesf"), ("A", "dg", di)], [PSK[7]])
                    if tb % 4 == 3:
                        t = tb // 4
                        cp("act", cwbs[ci][:, t * TW:(t + 1) * TW], ps[7][:, :], [PSK[7]], [("A", "cwb", ci)])
                ffn_groups(exp_wg_d[0][e_], exp_wu_d[0][e_], exp_wd_d[0][e_], DFE, (cwbs[ci], ("A", "cwb", ci)), gcount)

        for s in range(nseq):
            fence()
            xs = [cv(i * 2048, 1024, F32) for i in range(2)]
            for tb in range(16):
                bi = tb % 2
                dma("sp", xs[bi], x_d[s, tb * 128:(tb + 1) * 128, :], slot("xs%d" % bi), [], [("A", "xs", bi)])
                for half in range(2):
                    bank = (2 * tb + half) % 2
                    pv = ps[bank][:, :].rearrange("p (k c) -> p k c", k=4)
                    for k4 in range(4):
                        kc = half * 4 + k4
                        tr(pv[:, k4, :], xs[bi][:, kc * 128:(kc + 1) * 128], [("A", "xs", bi), ("C", "ident")], [PSK[bank]])
                    cp("act" if half == 0 else "dve", xT[:, half * 4:half * 4 + 4, tb * 128:(tb + 1) * 128], pv, [PSK[bank]],
                       [("X", half * 4 + k4, tb // 4) for k4 in range(4)])
            for l in range(NL):
                base = l * 10
                if stop >= base + 1:
                    norm_to_h(l * 16)
                    fence()
                if stop >= base + 2:
                    mixer_a(l)
                    fence()
                if stop >= base + 3:
                    mixer_b(l)
                    fence()
                if stop >= base + 4:
                    mixer_c(l)
                    fence()
                if dbg == ("y", s, l):
                    dma("sp", dbg_d, arena[:, 0:24576], slot("dbg"), [("Y", "dump")], [("D", "dbg")])
                    fence()
                if stop >= base + 5:
                    gate_phase(l)
                    fence()
                if stop >= base + 6:
                    if l % 2 == 0:
                        ffn_dense(l)
                    else:
                        ffn_moe(l)
                    fence()
            onT = cv(0, 4096, F32).rearrange("p (k t) -> p k t", k=8)
            osb = [cv(8192 + i * 2048, 1024, F32) for i in range(2)]
            for t in range(NT):
                rmsnorm_tile(t, 32, lambda kc: onT[:, kc, :], lambda kc: [("A", "onT", kc)], 2 + (t % 2))
                for j in range(4):
                    tb = t * 4 + j
                    oi = tb % 2
                    for half in range(2):
                        bank = half
                        pv = ps[bank][:, :].rearrange("p (k c) -> p k c", k=4)
                        for k4 in range(4):
                            kc = half * 4 + k4
                            tr(pv[:, k4, :], onT[:, kc, j * 128:(j + 1) * 128], [("A", "onT", kc), ("C", "ident")], [PSK[bank]])
                        cp("act" if half == 0 else "dve", osb[oi][:, half * 512:(half + 1) * 512], ps[bank][:, :], [PSK[bank]],
                           [("A", "osb", oi, half)])
                    dma("sp", out_d[s, tb * 128:(tb + 1) * 128, :], osb[oi], slot("out%d" % oi),
                        [("A", "osb", oi, 0), ("A", "osb", oi, 1)], [("D", "out", s, tb)])
        final_keys = [("D", "out", s, tb) for s in range(nseq) for tb in range(16)]
        if dbg is not None:
            final_keys.append(("D", "dbg"))
        P.wait_all("sp", final_keys)
        P.emit()
    return nc


class KeyList(tuple):
    pass


def _flat(keys):
    out = []
    for k in keys:
        if isinstance(k, KeyList):
            out.extend(k)
        else:
            out.append(k)
    return out


def _host_consts(inp):
    rope, oh, mult, ident = _const_tables()
    gains = np.zeros((128, 40), np.float32)
    for l in range(NL):
        gains[:, l * 16:l * 16 + 8] = np.asarray(inp["norm1_g"][l], np.float32).reshape(8, 128).T
        gains[:, l * 16 + 8:l * 16 + 16] = np.asarray(inp["norm2_g"][l], np.float32).reshape(8, 128).T
    gains[:, 32:40] = np.asarray(inp["final_g"], np.float32).reshape(8, 128).T
    qg = np.zeros((128, 4), np.float32)
    kvg = np.zeros((128, 2), np.float32)
    sinkb = np.zeros((128, 16), np.float32)
    for l in range(NL):
        qg[:, 2 * l:2 * l + 2] = np.asarray(inp["q_norm_g"][l], np.float32).reshape(2, 128).T
        kvg[:, l] = np.asarray(inp["kv_norm_g"][l], np.float32)
        sinkb[:, l * 8:(l + 1) * 8] = np.asarray(inp["sink_logit"][l], np.float32)[None, :]
    return dict(gains=gains, qg=qg, kvg=kvg, sinkb=sinkb, c_rope=rope, c_oh=oh, c_mult=mult, c_ident=ident)


_W_NAMES = ["w_in", "w_uq", "w_ukv", "rel_bias", "w_branch_a", "w_branch_b", "w_branch_c", "w_out",
            "ffn_w_gate", "ffn_w_up", "ffn_w_down", "router_w", "exp_w_gate", "exp_w_up", "exp_w_down"]


def kernel(**inputs):
    inp = {k: np.asarray(v) for k, v in inputs.items()}
    consts = _host_consts(inp)
    shared = {k: np.ascontiguousarray(inp[k], dtype=np.float32) for k in _W_NAMES}
    shared.update(consts)
    x = np.ascontiguousarray(inp["x"], dtype=np.float32)
    nc = build(NSEQ)
    in_maps = []
    for c in range(NCORES):
        m = dict(shared)
        m["x"] = x[c * NSEQ:(c + 1) * NSEQ]
        in_maps.append(m)
    res = run_bass_kernel_spmd(nc, in_maps, core_ids=list(range(NCORES)))
    out = np.concatenate([np.asarray(r["out"]) for r in res.results], axis=0)
    return out.astype(np.float32)
```

```python
import math
from contextlib import ExitStack

import numpy as np
import concourse.bass as bass
import concourse.mybir as mybir
from concourse.bass_utils import run_bass_kernel_spmd

F32 = mybir.dt.float32
BF16 = mybir.dt.bfloat16
AF = mybir.ActivationFunctionType
ALU = mybir.AluOpType
AX = mybir.AxisListType

S = 2048
D = 1024
KC = 8
NT = 4
TW = 512
NL = 2
A_COLS = 416
B0 = 416
C0 = 1952
G0 = 2720
IN_COLS = 5792
DFF = 2816
NE = 8
DFE = 3584
GW = 3072
D0 = 1536
HW = 2945
HWP = 2946
EPS = 1e-6
NCORES = 8
NSEQ = 2

ENGS = ["pe", "act", "dve", "pool", "sp"]
SEM_ROLL = 24000
ARENA_GROUPS = ("Y", "A")
TAB_ENG = "dve"
QK_REP = 1
KR_BC = 128
EVAC = "dve"


class Slot:
    def __init__(self, prog, name):
        self.sem = prog._new_sem("s_" + name)
        self.count = 0


class Prog:
    def __init__(self, nc, stack):
        self.nc = nc
        self.stack = stack
        self.sems = []
        self.ops = {e: [] for e in ENGS}
        self.eng_sem = {}
        self.eng_cnt = {}
        for e in ENGS:
            self.eng_sem[e] = self._new_sem("e_" + e)
            self.eng_cnt[e] = 0
        self.state = {}
        self.waited = {}
        self.fence = {}
        self.nroll = 0

    def _new_sem(self, name):
        s = self.stack.enter_context(self.nc.semaphore(name))
        self.sems.append(s)
        return len(self.sems) - 1

    def _deps(self, eng, reads, writes):
        deps = []
        for k in reads:
            st = self.state.get(k)
            if st is None:
                if k[0] in ARENA_GROUPS:
                    for t in self.fence.get("ARENA", ()):
                        deps.append((t, "raw"))
            elif st[0] is not None:
                deps.append((st[0], "raw"))
        for k in writes:
            st = self.state.get(k)
            if st is None:
                if k[0] in ARENA_GROUPS:
                    for t in self.fence.get("ARENA", ()):
                        deps.append((t, "raw"))
            else:
                if st[0] is not None:
                    deps.append((st[0], "waw"))
                for r in st[1]:
                    deps.append((r, "war"))
        waits = []
        for tok, kind in deps:
            semid, val, teng, is_dma = tok
            if (not is_dma) and teng == eng:
                if eng == "pe":
                    continue
            key = (eng, semid)
            if self.waited.get(key, 0) >= val:
                continue
            self.waited[key] = val
            waits.append((semid, val))
        return waits

    def _update(self, tok, reads, writes):
        for k in reads:
            st = self.state.get(k)
            if st is None:
                st = self.state[k] = [None, []]
            st[1].append(tok)
            if len(st[1]) > 24:
                best = {}
                for t in st[1]:
                    if t[0] not in best or best[t[0]][1] < t[1]:
                        best[t[0]] = t
                st[1] = list(best.values())
        for k in writes:
            self.state[k] = [tok, []]

    def do_fence(self, groups):
        best = {}
        for t in self.fence.get("ARENA", ()):
            best[t[0]] = t
        dead = [k for k in self.state if k[0] in ARENA_GROUPS]
        for k in dead:
            st = self.state.pop(k)
            toks = list(st[1])
            if st[0] is not None:
                toks.append(st[0])
            for t in toks:
                if t[0] not in best or best[t[0]][1] < t[1]:
                    best[t[0]] = t
        self.fence["ARENA"] = list(best.values())

    def op(self, eng, fn, reads=(), writes=()):
        waits = self._deps(eng, reads, writes)
        if self.eng_cnt[eng] >= SEM_ROLL:
            self.nroll += 1
            self.eng_sem[eng] = self._new_sem("e_%s_%d" % (eng, self.nroll))
            self.eng_cnt[eng] = 0
        self.eng_cnt[eng] += 1
        tok = (self.eng_sem[eng], self.eng_cnt[eng], eng, False)
        self.ops[eng].append((fn, waits, self.eng_sem[eng], 1))
        self._update(tok, reads, writes)

    def dma(self, eng, fn, slot, reads=(), writes=()):
        waits = self._deps(eng, reads, writes)
        slot.count += 16
        tok = (slot.sem, slot.count, eng, True)
        self.ops[eng].append((fn, waits, slot.sem, 16))
        self._update(tok, reads, writes)

    def wait_all(self, eng, keys):
        waits = self._deps(eng, keys, ())
        self.ops[eng].append((None, waits, None, 0))

    def emit(self):
        nc = self.nc
        sems = self.sems
        ops = self.ops
        with nc.Block() as block:
            def run(e, lst):
                for fn, waits, semid, inc in lst:
                    for (sid, val) in waits:
                        e.wait_ge(sems[sid], val)
                    if fn is None:
                        continue
                    fn(e).then_inc(sems[semid], inc)

            @block.tensor
            def _(e):
                run(e, ops["pe"])

            @block.scalar
            def _(e):
                run(e, ops["act"])

            @block.vector
            def _(e):
                run(e, ops["dve"])

            @block.gpsimd
            def _(e):
                run(e, ops["pool"])

            @block.sync
            def _(e):
                run(e, ops["sp"])


def _t5_bucket(rel):
    nb = 16
    max_exact = 8
    ret = np.where(rel > 0, nb, 0)
    n = np.abs(rel)
    nf = np.maximum(n, 1).astype(np.float32)
    large = max_exact + (np.log(nf / np.float32(max_exact)) / np.float32(math.log(1024 / max_exact))
                         * np.float32(nb - max_exact)).astype(np.int32)
    large = np.minimum(large, nb - 1)
    return ret + np.where(n < max_exact, n, large)


def _const_tables():
    half = 16
    inv = (np.float32(10000.0) ** (-np.arange(half, dtype=np.float32) / np.float32(half))).astype(np.float32)
    ang = np.arange(S, dtype=np.float32)[:, None] * inv[None, :]
    cos = np.cos(ang).astype(np.float32).T
    sin = np.sin(ang).astype(np.float32).T
    rope = np.ones((128, S), np.float32)
    rope[64:80] = cos
    rope[80:96] = cos
    rope[96:112] = -sin
    rope[112:128] = sin
    delta = np.arange(GW, dtype=np.int64) - D0
    bk = _t5_bucket(delta.astype(np.int32))
    oh = np.zeros((32, GW), np.float32)
    oh[bk, np.arange(GW)] = 1.0
    ad = np.abs(delta)
    mb = ((ad <= 64).astype(np.float32) + ((delta % 4 == 0) & (ad <= 256)).astype(np.float32)
          + ((delta % 16 == 0) & (ad <= 1024)).astype(np.float32))
    mc = (ad <= 128).astype(np.float32)
    mult = np.zeros((16, GW), np.float32)
    mult[:8] = mb[None]
    mult[8:] = mc[None]
    ident = np.eye(128, dtype=np.float32)
    return rope, oh, mult, ident


def build(nseq=NSEQ, dbg=None, stop=99, do_tables=True, xp=0, flags=()):
    nc = bass.Bass("TRN2", target_bir_lowering=False)

    def din(name, shape, dt=F32):
        return nc.dram_tensor(name, list(shape), dt, kind="ExternalInput").ap()

    x_d = din("x", [nseq, S, D])
    w_in_d = din("w_in", [NL, D, IN_COLS])
    w_uq_d = din("w_uq", [NL, 256, 768])
    w_ukv_d = din("w_ukv", [NL, 128, 1024])
    rel_bias_d = din("rel_bias", [32, 16])
    w_br_d = [din("w_branch_a", [NL, 512, D]), din("w_branch_b", [NL, 512, D]), din("w_branch_c", [NL, 512, D])]
    w_out_d = din("w_out", [NL, D, D])
    ffn_wg_d = din("ffn_w_gate", [1, D, DFF])
    ffn_wu_d = din("ffn_w_up", [1, D, DFF])
    ffn_wd_d = din("ffn_w_down", [1, DFF, D])
    router_d = din("router_w", [1, D, NE])
    exp_wg_d = din("exp_w_gate", [1, NE, D, DFE])
    exp_wu_d = din("exp_w_up", [1, NE, D, DFE])
    exp_wd_d = din("exp_w_down", [1, NE, DFE, D])
    gains_d = din("gains", [128, 40])
    qg_d = din("qg", [128, 4])
    kvg_d = din("kvg", [128, 2])
    sinkb_d = din("sinkb", [128, 16])
    c_rope_d = din("c_rope", [128, S])
    c_oh_d = din("c_oh", [32, GW])
    c_mult_d = din("c_mult", [16, GW])
    c_ident_d = din("c_ident", [128, 128])
    out_d = nc.dram_tensor("out", [nseq, S, D], F32, kind="ExternalOutput").ap()
    gtab_d = nc.dram_tensor("gtab", [16, GW], BF16, kind="Internal").ap()
    dbg_d = None
    if dbg is not None:
        dbg_d = nc.dram_tensor("dbg", [128, 12 * S], BF16, kind="ExternalOutput").ap()

    with ExitStack() as st:
        P = Prog(nc, st)

        def sb(name, shape, dt):
            return st.enter_context(nc.sbuf_tensor(name, list(shape), dt))

        xT = sb("xT", [128, KC, S], F32)
        hT = sb("hT", [128, KC, S], BF16)
        arena = sb("arena", [128, 49152], BF16)
        wx = sb("wx", [128, 7168], BF16)
        ident = sb("ident", [128, 128], F32)
        onesb = sb("onesb", [128, 128], BF16)
        onesf = sb("onesf", [128, 128], F32)
        cst = sb("cst", [128, 8], F32)
        gains = sb("gains_sb", [128, 40], F32)
        qg = sb("qg_sb", [128, 4], F32)
        kvg = sb("kvg_sb", [128, 2], F32)
        esink = sb("esink", [128, 16], F32)
        ps = [st.enter_context(nc.psum_tensor("ps%d" % i, [128, 512], F32)) for i in range(8)]

        def cv(off, n, dt=BF16):
            if dt == BF16:
                return arena[:, off:off + n]
            return arena[:, off:off + 2 * n].bitcast(F32)

        slots = {}

        def slot(name):
            if name not in slots:
                slots[name] = Slot(P, name)
            return slots[name]

        PSK = [("PS", i) for i in range(8)]

        def mm(out, lhsT, rhs, start, stop, reads, writes):
            P.op("pe", lambda e: e.matmul(out, lhsT=lhsT, rhs=rhs, start=start, stop=stop), reads, writes)

        def tr(out, in_, reads, writes):
            P.op("pe", lambda e: e.transpose(out, in_, ident[:]), reads, writes)

        def act(out, in_, func, reads, writes, scale=None, bias=None):
            kw = {}
            if scale is not None:
                kw["scale"] = scale
            if bias is not None:
                kw["bias"] = bias
            P.op("act", lambda e: e.activation(out=out, in_=in_, func=func, **kw), reads, writes)

        def cp(eng, out, in_, reads, writes):
            if eng == "act":
                P.op("act", lambda e: e.activation(out=out, in_=in_, func=AF.Copy), reads, writes)
            else:
                P.op(eng, lambda e: e.tensor_copy(out=out, in_=in_), reads, writes)

        def tt(out, in0, in1, op, reads, writes, eng="dve"):
            P.op(eng, lambda e: e.tensor_tensor(out=out, in0=in0, in1=in1, op=op), reads, writes)

        def ts(out, in0, s1, op0, reads, writes, s2=None, op1=None, eng="dve"):
            if op1 is None:
                P.op(eng, lambda e: e.tensor_scalar(out=out, in0=in0, scalar1=s1, scalar2=None, op0=op0), reads, writes)
            else:
                P.op(eng, lambda e: e.tensor_scalar(out=out, in0=in0, scalar1=s1, scalar2=s2, op0=op0, op1=op1),
                     reads, writes)

        def stt(out, in0, scalar, in1, op0, op1, reads, writes):
            P.op("dve", lambda e: e.scalar_tensor_tensor(out=out, in0=in0, scalar=scalar, in1=in1, op0=op0, op1=op1),
                 reads, writes)

        def recip(out, in_, reads, writes):
            P.op("dve", lambda e: e.reciprocal(out=out, in_=in_), reads, writes)

        def memset(eng, ap, val, writes):
            P.op(eng, lambda e: e.memset(ap, val), (), writes)

        def dma(eng, out, in_, sl, reads, writes):
            P.dma(eng, lambda e: e.dma_start(out=out, in_=in_), sl, reads, writes)

        def fence():
            P.do_fence(["Y", "A"])

        memset("dve", onesb[:], 1.0, [("C", "onesb")])
        memset("dve", onesf[:], 1.0, [("C", "onesf")])
        memset("dve", cst[:, 0:1], EPS, [("C", "cst")])
        dma("sp", ident[:], c_ident_d, slot("ident"), [], [("C", "ident")])
        dma("sp", gains[:], gains_d, slot("gains"), [], [("C", "gains")])
        dma("sp", qg[:], qg_d, slot("qg"), [], [("C", "qg")])
        dma("sp", kvg[:], kvg_d, slot("kvg"), [], [("C", "kvg")])
        dma("sp", esink[:], sinkb_d, slot("esink"), [], [("C", "esink0")])
        act(esink[:], esink[:], AF.Exp, [("C", "esink0")], [("C", "esink")])

        oh_sb = cv(0, 3072, F32)
        mult_sb = cv(6144, 3072, F32)
        gexp_sb = cv(12288, 3072, F32)
        gw_sb = cv(18432, 3072, BF16)
        rb_sb = cv(22016, 16, F32)
        dma("sp", oh_sb[0:32, :], c_oh_d, slot("oh"), [], [("A", "oh")])
        dma("sp", mult_sb[0:16, :], c_mult_d, slot("mult"), [], [("A", "mult")])
        dma("sp", rb_sb[0:32, :], rel_bias_d, slot("rb"), [], [("A", "rb")])
        for j in range(GW // 512):
            mm(ps[6][0:16, :], rb_sb[0:32, :], oh_sb[0:32, j * 512:(j + 1) * 512], True, True,
               [("A", "oh"), ("A", "rb")], [PSK[6]])
            act(gexp_sb[0:16, j * 512:(j + 1) * 512], ps[6][0:16, :], AF.Exp, [PSK[6]], [("A", "gexp", j)])
            tt(gw_sb[0:16, j * 512:(j + 1) * 512], gexp_sb[0:16, j * 512:(j + 1) * 512],
               mult_sb[0:16, j * 512:(j + 1) * 512], ALU.mult, [("A", "gexp", j), ("A", "mult")], [("A", "gw", j)])
        dma("sp", gtab_d, gw_sb[0:16, :], slot("gtab"), [("A", "gw", j) for j in range(GW // 512)], [("D", "gtab")])
        fence()

        yT = arena[:, 0:24576].rearrange("p (c s) -> p c s", c=12)
        U = 24576
        Qb = [cv(U + i * 2048, 2048) for i in range(2)]
        Kb = [cv(U + 4096 + i * 2048, 2048) for i in range(2)]
        Vb = [cv(U + 8192 + i * 2048, 2048).rearrange("p (b c) -> p b c", b=16) for i in range(2)]
        Pt = [cv(U + 12288 + i * 512, 512) for i in range(3)] + [cv(U + 14848, 512)]
        SB = [0, 1, 2, 7]
        rd = cv(U + 13824, 512, F32)
        dtmp = cv(U + 14848, 512, F32)
        R = U + 15872
        TOP = 40960
        sq = cv(TOP, 4096).rearrange("p (k t) -> p k t", k=8)
        sd = cv(TOP + 4096, 512, F32)
        rstd = cv(TOP + 5120, 512, F32)
        RT = TOP + 6144

        def rmsnorm_tile(t, gcol, out_fn, out_keys_fn, nbank):
            tsl = slice(t * TW, (t + 1) * TW)
            xk = [("X", kc, t) for kc in range(KC)]
            act(sq, xT[:, :, tsl], AF.Square, xk, [("A", "sq")])
            for kc in range(KC):
                mm(ps[nbank][:, :], onesb[:], sq[:, kc, :], kc == 0, kc == KC - 1,
                   [("A", "sq"), ("C", "onesb")], [PSK[nbank]])
            act(sd, ps[nbank][:, :], AF.Sqrt, [PSK[nbank], ("C", "cst")], [("A", "sd")], scale=1.0 / D, bias=cst[:, 0:1])
            recip(rstd, sd, [("A", "sd")], [("A", "rstd")])
            for kc in range(KC):
                stt(out_fn(kc), xT[:, kc, tsl], gains[:, gcol + kc:gcol + kc + 1], rstd, ALU.mult, ALU.mult,
                    [("X", kc, t), ("A", "rstd"), ("C", "gains")], out_keys_fn(kc))

        def norm_to_h(gcol):
            for t in range(NT):
                tsl = slice(t * TW, (t + 1) * TW)
                rmsnorm_tile(t, gcol, lambda kc: hT[:, kc, tsl], lambda kc: [("H", kc, t)], 2 + (t % 2))

        def attention(Q, K, V, Kr, lo, hi, table, tkey, scale, sinkcol, h, qkeys, kkeys, vkey, ychunk0, fillers=()):
            parity = h % 2
            nr = slice(64, 128) if parity else slice(0, 64)
            dr = slice(0, 64) if parity else slice(64, 128)
            chunk = ychunk0 + h // 2
            DEPTH = 3
            fillers = list(fillers)
            nfill = len(fillers)
            steps = []
            for qt in range(NT):
                kbs = [kb for kb in range(16) if lo <= kb - 4 * qt <= hi]
                for i, kb in enumerate(kbs):
                    steps.append((qt, kb, i == 0, i == len(kbs) - 1))
            total_steps = len(steps)
            queue = []
            fdone = [0]

            def finalize(qt):
                qsl = slice(qt * TW, (qt + 1) * TW)
                ob = 3 + (qt % 2)
                if sinkcol is not None:
                    ts(rd[nr, :], ps[ob][dr, :], esink[dr, sinkcol:sinkcol + 1], ALU.add,
                       [PSK[ob], ("C", "esink")], [("A", "rd")])
                    recip(rd[nr, :], rd[nr, :], [("A", "rd")], [("A", "rd")])
                else:
                    recip(rd[nr, :], ps[ob][dr, :], [PSK[ob]], [("A", "rd")])
                tt(yT[nr, chunk, qsl], ps[ob][nr, :], rd[nr, :], ALU.mult, [PSK[ob], ("A", "rd")],
                   [("Y", chunk, qt, parity)])

            def pv_one():
                pb_, pkb, pqt, first, last = queue.pop(0)
                ob = 3 + (pqt % 2)
                mm(ps[ob][:, :], V[:, pkb, :], Pt[pb_], first, last, [("A", "Pt", pb_), vkey], [PSK[ob]])
                if last:
                    finalize(pqt)

            for si, (qt, kb, first, last) in enumerate(steps):
                qsl = slice(qt * TW, (qt + 1) * TW)
                b = si % 4
                sb_ = SB[b]
                mm(ps[sb_][:, :], K[0:Kr, kb * 128:(kb + 1) * 128], Q[0:Kr, qsl], True, True,
                   _flat([qkeys[qt], kkeys[kb // 4]]), [PSK[sb_]])
                act(Pt[b], ps[sb_][:, :], AF.Exp, [PSK[sb_]], [("A", "Pt", b)], scale=scale)
                if table is not None:
                    u0 = D0 + 128 * (kb - 4 * qt)
                    tt(Pt[b], Pt[b], table[:, u0:u0 - 512:-1], ALU.mult, [("A", "Pt", b), tkey], [("A", "Pt", b)],
                       eng=TAB_ENG)
                queue.append((b, kb, qt, first, last))
                if len(queue) > DEPTH:
                    pv_one()
                while fdone[0] < nfill and fdone[0] * total_steps < (si + 1) * nfill:
                    fillers[fdone[0]]()
                    fdone[0] += 1
            while queue:
                pv_one()
            while fdone[0] < nfill:
                fillers[fdone[0]]()
                fdone[0] += 1

        def mixer_a(l):
            rope = arena[:, 8192:12288].bitcast(F32)
            qnT = arena[:, 12288:16384].rearrange("p (c s) -> p c s", c=2)
            kvnT = arena[:, 16384:18432]
            sqA = arena[:, 18432:19968].rearrange("p (c t) -> p c t", c=3)
            sdA = arena[:, 19968:22016].bitcast(F32).rearrange("p (c t) -> p c t", c=2)
            kt1 = arena[:, 22016:23040].bitcast(F32)
            kt2 = arena[:, 23040:24064].bitcast(F32)
            Wq = cv(R, 2048).rearrange("p (k h c) -> p k h c", k=2, h=8)
            Wkv = cv(R + 2048, 1024)
            Wa = cv(R + 3072, 3072).rearrange("p (k c) -> p k c", k=8)
            Wkpe = cv(R + 6144, 1024).rearrange("p (k c) -> p k c", k=8)
            win = w_in_d[l].rearrange("(k p) c -> p k c", p=128)
            memset("pool", Vb[0][:, :, 64:128], 1.0, [("A", "V", 0)])
            memset("pool", Vb[1][:, :, 0:64], 1.0, [("A", "V", 1)])
            dma("sp", rope, c_rope_d, slot("rope"), [], [("Y", "rope")])
            dma("pool", Wa, win[:, :, 0:384], slot("Wa"), [], [("A", "Wa")])
            dma("pool", Wkpe[:, :, 0:96], win[:, :, 320:416], slot("Wkpe"), [], [("A", "Wkpe")])
            dma("pool", Wkpe[:, :, 96:112], win[:, :, 400:416], slot("Wkpe"), [], [("A", "Wkpe")])
            dma("pool", Wkpe[:, :, 112:128], win[:, :, 384:400], slot("Wkpe"), [], [("A", "Wkpe")])
            wuq = w_uq_d[l].rearrange("(k p) (h c) -> p k h c", p=128, h=8)
            for kc in range(2):
                dma("pool", Wq[:, kc, :, 0:96], wuq[:, kc, :, :], slot("Wq"), [], [("A", "Wq")])
                dma("pool", Wq[:, kc, :, 96:112], wuq[:, kc, :, 80:96], slot("Wq"), [], [("A", "Wq")])
                dma("pool", Wq[:, kc, :, 112:128], wuq[:, kc, :, 64:80], slot("Wq"), [], [("A", "Wq")])
            dma("pool", Wkv, w_ukv_d[l], slot("Wkv"), [], [("A", "Wkv")])
            for t in range(NT):
                tsl = slice(t * TW, (t + 1) * TW)
                hk = [("H", kc, t) for kc in range(KC)]
                for c in range(3):
                    for kc in range(KC):
                        mm(ps[c][:, :], Wa[:, kc, c * 128:(c + 1) * 128], hT[:, kc, tsl], kc == 0, kc == KC - 1,
                           [("A", "Wa"), hk[kc]], [PSK[c]])
                for kc in range(KC):
                    mm(ps[3][:, :], Wkpe[:, kc, :], hT[:, kc, tsl], kc == 0, kc == KC - 1, [("A", "Wkpe"), hk[kc]], [PSK[3]])
                for c in range(3):
                    act(sqA[:, c, :], ps[c][:, :], AF.Square, [PSK[c]], [("Y", "sqA", c)])
                for c in range(2):
                    mm(ps[4][:, :], onesb[:], sqA[:, c, :], c == 0, c == 1, [("Y", "sqA", c), ("C", "onesb")], [PSK[4]])
                mm(ps[5][:, :], onesb[:], sqA[:, 2, :], True, True, [("Y", "sqA", 2), ("C", "onesb")], [PSK[5]])
                act(sdA[:, 0, :], ps[4][:, :], AF.Sqrt, [PSK[4], ("C", "cst")], [("Y", "sdA", 0)], scale=1.0 / 256, bias=cst[:, 0:1])
                act(sdA[:, 1, :], ps[5][:, :], AF.Sqrt, [PSK[5], ("C", "cst")], [("Y", "sdA", 1)], scale=1.0 / 128, bias=cst[:, 0:1])
                recip(sdA[:, 0, :], sdA[:, 0, :], [("Y", "sdA", 0)], [("Y", "sdA", 0)])
                recip(sdA[:, 1, :], sdA[:, 1, :], [("Y", "sdA", 1)], [("Y", "sdA", 1)])
                for c in range(2):
                    stt(qnT[:, c, tsl], ps[c][:, :], qg[:, 2 * l + c:2 * l + c + 1], sdA[:, 0, :], ALU.mult, ALU.mult,
                        [PSK[c], ("Y", "sdA", 0), ("C", "qg")], [("Y", "qnT", c, t)])
                stt(kvnT[:, tsl], ps[2][:, :], kvg[:, l:l + 1], sdA[:, 1, :], ALU.mult, ALU.mult,
                    [PSK[2], ("Y", "sdA", 1), ("C", "kvg")], [("Y", "kvnT", t)])
                tt(kt1[64:128, :], ps[3][64:128, :], rope[64:128, tsl], ALU.mult, [PSK[3], ("Y", "rope")], [("Y", "kt1")])
                cp("act", kt2[64:96, :], kt1[96:128, :], [("Y", "kt1")], [("Y", "kt2")])
                tt(Kb[0][64:96, tsl], kt1[64:96, :], kt2[64:96, :], ALU.add, [("Y", "kt1"), ("Y", "kt2")], [("A", "Kpe", 0, t, 0)])
                cp("act", Kb[0][96:128, tsl], Kb[0][64:96, tsl], [("A", "Kpe", 0, t, 0)], [("A", "Kpe", 0, t, 1)])
                cp("act", Kb[1][64:96, tsl], Kb[0][64:96, tsl], [("A", "Kpe", 0, t, 0)], [("A", "Kpe", 1, t, 0)])
                cp("act", Kb[1][96:128, tsl], Kb[0][64:96, tsl], [("A", "Kpe", 0, t, 0)], [("A", "Kpe", 1, t, 1)])

            def proj_chunks(h):
                hb = h % 2
                voff = 64 if hb else 0
                chunks = []

                def mk_t(t):
                    def f():
                        tsl = slice(t * TW, (t + 1) * TW)
                        for kc in range(2):
                            mm(ps[5][:, :], Wq[:, kc, h, :], qnT[:, kc, tsl], kc == 0, kc == 1,
                               [("A", "Wq"), ("Y", "qnT", kc, t)], [PSK[5]])
                        tt(Qb[hb][:, tsl], ps[5][:, :], rope[:, tsl], ALU.mult, [PSK[5], ("Y", "rope")], [("A", "Q", hb, t)])
                        mm(ps[5][0:64, :], Wkv[:, h * 128:h * 128 + 64], kvnT[:, tsl], True, True,
                           [("A", "Wkv"), ("Y", "kvnT", t)], [PSK[5]])
                        cp(EVAC, Kb[hb][0:64, tsl], ps[5][0:64, :], [PSK[5]], [("A", "K", hb, t)])
                    return f

                def mk_v(half):
                    def f():
                        bank = 6
                        pv = ps[bank][:, :].rearrange("p (b c) -> p b c", b=8)
                        for j in range(8):
                            tb = half * 8 + j
                            mm(pv[:, j, :], kvnT[:, tb * 128:(tb + 1) * 128], Wkv[:, h * 128 + 64:h * 128 + 128], True, True,
                               [("A", "Wkv"), ("Y", "kvnT", tb // 4)], [PSK[bank]])
                        cp(EVAC, Vb[hb][:, half * 8:half * 8 + 8, voff:voff + 64], pv, [PSK[bank]], [("A", "V", hb)])
                    return f

                for t in range(NT):
                    chunks.append(mk_t(t))
                for half in range(2):
                    chunks.append(mk_v(half))
                return chunks

            for c in proj_chunks(0):
                c()
            for h in range(8):
                hb = h % 2
                attention(Qb[hb], Kb[hb], Vb[hb], 128, -100, 100, None, None, 96.0 ** -0.5, None, h,
                          [("A", "Q", hb, t) for t in range(NT)],
                          [KeyList([("A", "K", hb, t), ("A", "Kpe", hb, t, 0), ("A", "Kpe", hb, t, 1)]) for t in range(NT)],
                          ("A", "V", hb), 0, fillers=proj_chunks(h + 1) if h + 1 < 8 else ())

        def load_table(hidx, buf, tb_ap):
            if xp == 1:
                return
            src = bass.AP(gtab_d.tensor, hidx * GW, [[1, 128], [1, HW]])
            dma("sp", tb_ap, src, slot("tab%d" % buf), [("D", "gtab")], [("A", "tab", buf)])

        def mixer_b(l):
            Wb = cv(R, 1536).rearrange("p (k c) -> p k c", k=8)
            tabs = [cv(R + 1536 + i * HWP, HW) for i in range(2)]
            win = w_in_d[l].rearrange("(k p) c -> p k c", p=128)

            def proj_chunks(h):
                hb = h % 2
                voff = 64 if hb else 0
                chunks = []

                def c_load():
                    for w in range(3):
                        c0 = B0 + w * 512 + h * 64
                        dma("pool", Wb[:, :, w * 64:(w + 1) * 64], win[:, :, c0:c0 + 64], slot("Wb"), [], [("A", "Wb")])
                    load_table(h, hb, tabs[hb])
                chunks.append(c_load)

                def mk_qk(t, w, dst, nm):
                    def f():
                        tsl = slice(t * TW, (t + 1) * TW)
                        for kc in range(KC):
                            mm(ps[5][0:64, :], Wb[:, kc, w * 64:(w + 1) * 64], hT[:, kc, tsl], kc == 0, kc == KC - 1,
                               [("A", "Wb"), ("H", kc, t)], [PSK[5]])
                        cp(EVAC, dst[hb][0:64, tsl], ps[5][0:64, :], [PSK[5]], [("A", nm, hb, t)])
                    return f

                def mk_v(half, j):
                    def f():
                        bank = 6
                        pv = ps[bank][:, :].rearrange("p (b c) -> p b c", b=8)
                        tb = half * 8 + j
                        for kc in range(KC):
                            mm(pv[:, j, :], hT[:, kc, tb * 128:(tb + 1) * 128], Wb[:, kc, 128:192], kc == 0, kc == KC - 1,
                               [("A", "Wb"), ("H", kc, tb // 4)], [PSK[bank]])
                        if j == 7:
                            cp(EVAC, Vb[hb][:, half * 8:half * 8 + 8, voff:voff + 64], pv, [PSK[bank]], [("A", "V", hb)])
                    return f

                for t in range(NT):
                    chunks.append(mk_qk(t, 0, Qb, "Q"))
                    chunks.append(mk_qk(t, 1, Kb, "K"))
                for half in range(2):
                    for j in range(8):
                        chunks.append(mk_v(half, j))
                return chunks

            for i in range(2):
                memset("pool", Qb[i][64:128, :], 0.0, [("A", "Qz", i)])
                memset("pool", Kb[i][64:128, :], 0.0, [("A", "Kz", i)])
            for c in proj_chunks(0):
                c()
            for h in range(8):
                hb = h % 2
                attention(Qb[hb], Kb[hb], Vb[hb], KR_BC, -8, 11, tabs[hb] if xp in (0, 3) else None, ("A", "tab", hb), 0.125, None, h,
                          [KeyList([("A", "Q", hb, t), ("A", "Qz", hb)]) for t in range(NT)],
                          [KeyList([("A", "K", hb, t), ("A", "Kz", hb)]) for t in range(NT)],
                          ("A", "V", hb), 4, fillers=proj_chunks(h + 1) if h + 1 < 8 else ())

        def mixer_c(l):
            Wcq = cv(R, 512).rearrange("p (k c) -> p k c", k=8)
            Wckv = cv(R + 512, 1024).rearrange("p (k c) -> p k c", k=8)
            tabs = [cv(R + 1536 + i * HWP, HW) for i in range(2)]
            win = w_in_d[l].rearrange("(k p) c -> p k c", p=128)

            def proj_kv(g):
                ck = C0 + 512 + g * 64
                cvv = C0 + 640 + g * 64
                dma("pool", Wckv[:, :, 0:64], win[:, :, ck:ck + 64], slot("Wckv"), [], [("A", "Wckv")])
                dma("pool", Wckv[:, :, 64:128], win[:, :, cvv:cvv + 64], slot("Wckv"), [], [("A", "Wckv")])
                for t in range(NT):
                    tsl = slice(t * TW, (t + 1) * TW)
                    for kc in range(KC):
                        mm(ps[5][0:64, :], Wckv[:, kc, 0:64], hT[:, kc, tsl], kc == 0, kc == KC - 1,
                           [("A", "Wckv"), ("H", kc, t)], [PSK[5]])
                    cp(EVAC, Kb[g][0:64, tsl], ps[5][0:64, :], [PSK[5]], [("A", "K", g, t)])
                for half in range(2):
                    bank = 6
                    pv = ps[bank][:, :].rearrange("p (b c) -> p b c", b=8)
                    for j in range(8):
                        tb = half * 8 + j
                        for kc in range(KC):
                            mm(pv[:, j, :], hT[:, kc, tb * 128:(tb + 1) * 128], Wckv[:, kc, 64:128], kc == 0, kc == KC - 1,
                               [("A", "Wckv"), ("H", kc, tb // 4)], [PSK[bank]])
                    cp(EVAC, Vb[0][:, half * 8:half * 8 + 8, 0:64], pv, [PSK[bank]], [("A", "V", 0)])
                    cp("pool", Vb[1][:, half * 8:half * 8 + 8, 64:128], Vb[0][:, half * 8:half * 8 + 8, 0:64],
                       [("A", "V", 0)], [("A", "V", 1)])

            def projq_chunks(h):
                hb = h % 2
                cq = C0 + h * 64
                chunks = []

                def c_load():
                    dma("pool", Wcq, win[:, :, cq:cq + 64], slot("Wcq"), [], [("A", "Wcq")])
                    load_table(8 + h, hb, tabs[hb])
                chunks.append(c_load)

                def mk_t(t):
                    def f():
                        tsl = slice(t * TW, (t + 1) * TW)
                        for kc in range(KC):
                            mm(ps[5][0:64, :], Wcq[:, kc, :], hT[:, kc, tsl], kc == 0, kc == KC - 1,
                               [("A", "Wcq"), ("H", kc, t)], [PSK[5]])
                        cp(EVAC, Qb[hb][0:64, tsl], ps[5][0:64, :], [PSK[5]], [("A", "Q", hb, t)])
                    return f

                for t in range(NT):
                    chunks.append(mk_t(t))
                return chunks

            for g in range(2):
                proj_kv(g)
                for c in projq_chunks(4 * g):
                    c()
                for r in range(4):
                    h = 4 * g + r
                    hb = h % 2
                    attention(Qb[hb], Kb[g], Vb[hb], KR_BC, -1, 4, tabs[hb], ("A", "tab", hb), 0.125, l * 8 + h, h,
                              [KeyList([("A", "Q", hb, t), ("A", "Qz", hb)]) for t in range(NT)],
                              [KeyList([("A", "K", g, t), ("A", "Kz", g)]) for t in range(NT)],
                              ("A", "V", hb), 8, fillers=projq_chunks(h + 1) if r + 1 < 4 else ())

        def gate_phase(l):
            merged = cv(U, 16384).rearrange("p (c s) -> p c s", c=8)
            mtemp = cv(U + 16384, 4096, F32).rearrange("p (c s) -> p c s", c=2)
            Wg = [wx[:, i * 2048:(i + 1) * 2048].rearrange("p (k c) -> p k c", k=8) for i in range(2)]
            Wbr = [wx[:, 4096 + i * 1024:4096 + (i + 1) * 1024].rearrange("p (k c) -> p k c", k=4) for i in range(2)]
            gsb = [wx[:, 6144 + i * 512:6144 + (i + 1) * 512] for i in range(2)]
            win = w_in_d[l].rearrange("(k p) c -> p k c", p=128)
            n = 0
            m = 0
            for cp in range(4):
                for b in range(3):
                    wi = n % 2
                    n += 1
                    col = G0 + b * 1024 + cp * 256
                    dma("pool", Wg[wi], win[:, :, col:col + 256], slot("Wg%d" % wi), [], [("W", "Wg", wi)])
                    wbr = w_br_d[b][l].rearrange("(k p) c -> p k c", p=128)
                    dma("pool", Wbr[wi], wbr[:, :, cp * 256:(cp + 1) * 256], slot("Wbr%d" % wi), [], [("W", "Wbr", wi)])
                    for cc in range(2):
                        c = 2 * cp + cc
                        csl = slice(cc * 128, (cc + 1) * 128)
                        for t in range(NT):
                            tsl = slice(t * TW, (t + 1) * TW)
                            gb = m % 2
                            m += 1
                            pg = ps[gb]
                            pb = ps[2 + gb]
                            for kc in range(KC):
                                mm(pg[:, :], Wg[wi][:, kc, csl], hT[:, kc, tsl], kc == 0, kc == KC - 1,
                                   [("W", "Wg", wi), ("H", kc, t)], [PSK[gb]])
                            act(gsb[gb], pg[:, :], AF.Sigmoid, [PSK[gb]], [("W", "gsb", gb)])
                            for kc in range(4):
                                mm(pb[:, :], Wbr[wi][:, kc, csl], yT[:, b * 4 + kc, tsl], kc == 0, kc == 3,
                                   [("W", "Wbr", wi), ("Y", b * 4 + kc, t, 0), ("Y", b * 4 + kc, t, 1)], [PSK[2 + gb]])
                            if b == 0:
                                tt(mtemp[:, cc, tsl], pb[:, :], gsb[gb], ALU.mult, [PSK[2 + gb], ("W", "gsb", gb)],
                                   [("A", "mtemp", cc, t)])
                            else:
                                tt(pb[:, :], pb[:, :], gsb[gb], ALU.mult, [PSK[2 + gb], ("W", "gsb", gb)], [PSK[2 + gb]])
                                if b == 1:
                                    tt(mtemp[:, cc, tsl], pb[:, :], mtemp[:, cc, tsl], ALU.add,
                                       [("A", "mtemp", cc, t), PSK[2 + gb]], [("A", "mtemp", cc, t)])
                                else:
                                    tt(merged[:, c, tsl], pb[:, :], mtemp[:, cc, tsl], ALU.add,
                                       [("A", "mtemp", cc, t), PSK[2 + gb]], [("A", "merged", c, t)])
            wo = w_out_d[l].rearrange("(k p) c -> p k c", p=128)
            for dcp in range(4):
                wi = n % 2
                n += 1
                dma("pool", Wg[wi], wo[:, :, dcp * 256:(dcp + 1) * 256], slot("Wg%d" % wi), [], [("W", "Wg", wi)])
                for cc in range(2):
                    dc = 2 * dcp + cc
                    csl = slice(cc * 128, (cc + 1) * 128)
                    for t in range(NT):
                        tsl = slice(t * TW, (t + 1) * TW)
                        ob = 4 + (t % 2)
                        for kc in range(KC):
                            mm(ps[ob][:, :], Wg[wi][:, kc, csl], merged[:, kc, tsl], kc == 0, kc == KC - 1,
                               [("W", "Wg", wi), ("A", "merged", kc, t)], [PSK[ob]])
                        tt(xT[:, dc, tsl], ps[ob][:, :], xT[:, dc, tsl], ALU.add, [PSK[ob], ("X", dc, t)], [("X", dc, t)])

        def ffn_groups(wg_ap, wu_ap, wd_ap, dff, cwb, gcount):
            WG = [cv(i * 4096, 4096).rearrange("p (k c) -> p k c", k=8) for i in range(2)]
            WU = [cv(8192 + i * 4096, 4096).rearrange("p (k c) -> p k c", k=8) for i in range(2)]
            WD = [cv(16384 + i * 4096, 4096).rearrange("p (j c) -> p j c", j=4) for i in range(2)]
            actb = cv(24576, 8192).rearrange("p (j s) -> p j s", j=4)
            sgt = [cv(32768 + i * 512, 512) for i in range(2)]
            tmpm = [cv(33792 + i * 512, 512) for i in range(2)]
            wgv = wg_ap.rearrange("(k p) c -> p k c", p=128)
            wuv = wu_ap.rearrange("(k p) c -> p k c", p=128)
            ngroups = (dff + 511) // 512
            for gi in range(ngroups):
                wi = gcount[0] % 2
                gcount[0] += 1
                c0 = gi * 512
                gw_ = min(512, dff - c0)
                nj = gw_ // 128
                dma("pool", WG[wi][:, :, 0:gw_], wgv[:, :, c0:c0 + gw_], slot("WG%d" % wi), [], [("A", "WG", wi)])
                dma("pool", WU[wi][:, :, 0:gw_], wuv[:, :, c0:c0 + gw_], slot("WU%d" % wi), [], [("A", "WU", wi)])
                wdv = wd_ap[c0:c0 + gw_, :].rearrange("(j p) c -> p j c", p=128)
                dma("pool", WD[wi][:, 0:nj, :], wdv, slot("WD%d" % wi), [], [("A", "WD", wi)])
                n = 0
                for t in range(NT):
                    tsl = slice(t * TW, (t + 1) * TW)
                    for j in range(nj):
                        gb = n % 2
                        n += 1
                        for kc in range(KC):
                            mm(ps[gb][:, :], WG[wi][:, kc, j * 128:(j + 1) * 128], hT[:, kc, tsl], kc == 0, kc == KC - 1,
                               [("A", "WG", wi), ("H", kc, t)], [PSK[gb]])
                        for kc in range(KC):
                            mm(ps[2 + gb][:, :], WU[wi][:, kc, j * 128:(j + 1) * 128], hT[:, kc, tsl], kc == 0, kc == KC - 1,
                               [("A", "WU", wi), ("H", kc, t)], [PSK[2 + gb]])
                        act(sgt[gb], ps[gb][:, :], AF.Silu, [PSK[gb]], [("A", "sgt", gb)])
                        if cwb is None:
                            tt(actb[:, j, tsl], ps[2 + gb][:, :], sgt[gb], ALU.mult, [PSK[2 + gb], ("A", "sgt", gb)],
                               [("A", "act", j, t)])
                        else:
                            tt(tmpm[gb], ps[2 + gb][:, :], sgt[gb], ALU.mult, [PSK[2 + gb], ("A", "sgt", gb)],
                               [("A", "tmpm", gb)])
                            tt(actb[:, j, tsl], tmpm[gb], cwb[0][:, tsl], ALU.mult, [("A", "tmpm", gb), cwb[1]],
                               [("A", "act", j, t)])
                for t in range(NT):
                    tsl = slice(t * TW, (t + 1) * TW)
                    for dc in range(8):
                        ob = 4 + (dc % 4)
                        for j in range(nj):
                            mm(ps[ob][:, :], WD[wi][:, j, dc * 128:(dc + 1) * 128], actb[:, j, tsl], j == 0, j == nj - 1,
                               [("A", "WD", wi), ("A", "act", j, t)], [PSK[ob]])
                        tt(xT[:, dc, tsl], ps[ob][:, :], xT[:, dc, tsl], ALU.add, [PSK[ob], ("X", dc, t)], [("X", dc, t)])

        def ffn_dense(l):
            norm_to_h(l * 16 + 8)
            fence()
            ffn_groups(ffn_wg_d[0], ffn_wu_d[0], ffn_wd_d[0], DFF, None, [0])

        def ffn_moe(l):
            h2f = arena[:, 16384:24576].bitcast(F32).rearrange("p (k t) -> p k t", k=8)
            wr = arena[:, 32768:32768 + 128].bitcast(F32).rearrange("p (k e) -> p k e", k=8)
            logits = arena[:, RT:RT + 256].bitcast(F32).rearrange("p (b e) -> p b e", b=16)
            l2 = arena[:, RT + 256:RT + 512].bitcast(F32).rearrange("p (b e) -> p b e", b=16)
            eq1 = arena[:, RT + 512:RT + 768].bitcast(F32).rearrange("p (b e) -> p b e", b=16)
            eq2 = arena[:, RT + 768:RT + 1024].bitcast(F32).rearrange("p (b e) -> p b e", b=16)
            comb = arena[:, RT + 1024:RT + 1280].bitcast(F32).rearrange("p (b e) -> p b e", b=16)
            sm = arena[:, RT + 1280:RT + 1280 + 32 * 8].bitcast(F32).rearrange("p (v b) -> p v b", v=8)
            m1, m2, dd, ee, g1, g2 = [sm[:, i, :] for i in range(6)]
            dma("sp", wr, router_d[0].rearrange("(k p) e -> p k e", p=128), slot("wr"), [], [("A", "wr")])
            for t in range(NT):
                tsl = slice(t * TW, (t + 1) * TW)
                rmsnorm_tile(t, l * 16 + 8, lambda kc: h2f[:, kc, :], lambda kc: [("A", "h2f", kc)], 2 + (t % 2))
                for kc in range(KC):
                    cp("act", hT[:, kc, tsl], h2f[:, kc, :], [("A", "h2f", kc)], [("H", kc, t)])
                for j in range(4):
                    tb = t * 4 + j
                    for kc in range(KC):
                        mm(ps[6][:, tb * 8:tb * 8 + 8], h2f[:, kc, j * 128:(j + 1) * 128], wr[:, kc, :], kc == 0, kc == KC - 1,
                           [("A", "h2f", kc), ("A", "wr")], [PSK[6]])
            pl = ps[6][:, 0:128].rearrange("p (b e) -> p b e", b=16)
            cp("dve", logits, pl, [PSK[6]], [("A", "logits")])
            P.op("dve", lambda e: e.tensor_reduce(out=m1, in_=logits, axis=AX.X, op=ALU.max), [("A", "logits")], [("A", "m1")])
            tt(eq1, logits, m1.unsqueeze(2).broadcast_to([128, 16, 8]), ALU.is_equal, [("A", "logits"), ("A", "m1")], [("A", "eq1")])
            stt(l2, eq1, -1e30, logits, ALU.mult, ALU.add, [("A", "eq1"), ("A", "logits")], [("A", "l2")])
            P.op("dve", lambda e: e.tensor_reduce(out=m2, in_=l2, axis=AX.X, op=ALU.max), [("A", "l2")], [("A", "m2")])
            tt(eq2, l2, m2.unsqueeze(2).broadcast_to([128, 16, 8]), ALU.is_equal, [("A", "l2"), ("A", "m2")], [("A", "eq2")])
            tt(dd, m2, m1, ALU.subtract, [("A", "m1"), ("A", "m2")], [("A", "dd")])
            act(ee, dd, AF.Exp, [("A", "dd")], [("A", "ee")])
            ts(dd, ee, 1.0, ALU.add, [("A", "ee")], [("A", "dd")])
            recip(g1, dd, [("A", "dd")], [("A", "g1")])
            tt(g2, ee, g1, ALU.mult, [("A", "ee"), ("A", "g1")], [("A", "g2")])
            tt(eq1, eq1, g1.unsqueeze(2).broadcast_to([128, 16, 8]), ALU.mult, [("A", "eq1"), ("A", "g1")], [("A", "eq1")])
            tt(eq2, eq2, g2.unsqueeze(2).broadcast_to([128, 16, 8]), ALU.mult, [("A", "eq2"), ("A", "g2")], [("A", "eq2")])
            tt(comb, eq1, eq2, ALU.add, [("A", "eq1"), ("A", "eq2")], [("C", "comb")])
            fence()
            cwbs = [cv(34816 + i * 2048, 2048) for i in range(2)]
            dg = [arena[:, 38912 + i * 256:38912 + (i + 1) * 256].bitcast(F32) for i in range(2)]
            gcount = [0]
            nd = 0
            for e_ in range(NE):
                ci = e_ % 2
                for tb in range(16):
                    di = nd % 2
                    nd += 1
                    ts(dg[di], ident[:], comb[:, tb, e_:e_ + 1], ALU.mult, [("C", "ident"), ("C", "comb")], [("A", "dg", di)])
                    mm(ps[7][:, (tb % 4) * 128:(tb % 4 + 1) * 128], onesf[:], dg[di], True, True,
                       [("C", "onesf"), ("A", "dg", di)], [PSK[7]])
                    if tb % 4 == 3:
                        t = tb // 4
                        cp("act", cwbs[ci][:, t * TW:(t + 1) * TW], ps[7][:, :], [PSK[7]], [("A", "cwb", ci)])
                ffn_groups(exp_wg_d[0][e_], exp_wu_d[0][e_], exp_wd_d[0][e_], DFE, (cwbs[ci], ("A", "cwb", ci)), gcount)

        for s in range(nseq):
            fence()
            xs = [cv(i * 2048, 1024, F32) for i in range(2)]
            for tb in range(16):
                bi = tb % 2
                dma("sp", xs[bi], x_d[s, tb * 128:(tb + 1) * 128, :], slot("xs%d" % bi), [], [("A", "xs", bi)])
                for half in range(2):
                    bank = (2 * tb + half) % 2
                    pv = ps[bank][:, :].rearrange("p (k c) -> p k c", k=4)
                    for k4 in range(4):
                        kc = half * 4 + k4
                        tr(pv[:, k4, :], xs[bi][:, kc * 128:(kc + 1) * 128], [("A", "xs", bi), ("C", "ident")], [PSK[bank]])
                    cp("act" if half == 0 else "dve", xT[:, half * 4:half * 4 + 4, tb * 128:(tb + 1) * 128], pv, [PSK[bank]],
                       [("X", half * 4 + k4, tb // 4) for k4 in range(4)])
            for l in range(NL):
                base = l * 10
                if stop >= base + 1:
                    norm_to_h(l * 16)
                    fence()
                if stop >= base + 2:
                    mixer_a(l)
                    fence()
                if stop >= base + 3:
                    mixer_b(l)
                    fence()
                if stop >= base + 4:
                    mixer_c(l)
                    fence()
                if dbg == ("y", s, l):
                    dma("sp", dbg_d, arena[:, 0:24576], slot("dbg"), [("Y", "dump")], [("D", "dbg")])
                    fence()
                if stop >= base + 5:
                    gate_phase(l)
                    fence()
                if stop >= base + 6:
                    if l % 2 == 0:
                        ffn_dense(l)
                    else:
                        ffn_moe(l)
                    fence()
            onT = cv(0, 4096, F32).rearrange("p (k t) -> p k t", k=8)
            osb = [cv(8192 + i * 2048, 1024, F32) for i in range(2)]
            for t in range(NT):
                rmsnorm_tile(t, 32, lambda kc: onT[:, kc, :], lambda kc: [("A", "onT", kc)], 2 + (t % 2))
                for j in range(4):
                    tb = t * 4 + j
                    oi = tb % 2
                    for half in range(2):
                        bank = half
                        pv = ps[bank][:, :].rearrange("p (k c) -> p k c", k=4)
                        for k4 in range(4):
                            kc = half * 4 + k4
                            tr(pv[:, k4, :], onT[:, kc, j * 128:(j + 1) * 128], [("A", "onT", kc), ("C", "ident")], [PSK[bank]])
                        cp("act" if half == 0 else "dve", osb[oi][:, half * 512:(half + 1) * 512], ps[bank][:, :], [PSK[bank]],
                           [("A", "osb", oi, half)])
                    dma("sp", out_d[s, tb * 128:(tb + 1) * 128, :], osb[oi], slot("out%d" % oi),
                        [("A", "osb", oi, 0), ("A", "osb", oi, 1)], [("D", "out", s, tb)])
        final_keys = [("D", "out", s, tb) for s in range(nseq) for tb in range(16)]
        if dbg is not None:
            final_keys.append(("D", "dbg"))
        P.wait_all("sp", final_keys)
        P.emit()
    return nc


class KeyList(tuple):
    pass


def _flat(keys):
    out = []
    for k in keys:
        if isinstance(k, KeyList):
            out.extend(k)
        else:
            out.append(k)
    return out


def _host_consts(inp):
    rope, oh, mult, ident = _const_tables()
    gains = np.zeros((128, 40), np.float32)
    for l in range(NL):
        gains[:, l * 16:l * 16 + 8] = np.asarray(inp["norm1_g"][l], np.float32).reshape(8, 128).T
        gains[:, l * 16 + 8:l * 16 + 16] = np.asarray(inp["norm2_g"][l], np.float32).reshape(8, 128).T
    gains[:, 32:40] = np.asarray(inp["final_g"], np.float32).reshape(8, 128).T
    qg = np.zeros((128, 4), np.float32)
    kvg = np.zeros((128, 2), np.float32)
    sinkb = np.zeros((128, 16), np.float32)
    for l in range(NL):
        qg[:, 2 * l:2 * l + 2] = np.asarray(inp["q_norm_g"][l], np.float32).reshape(2, 128).T
        kvg[:, l] = np.asarray(inp["kv_norm_g"][l], np.float32)
        sinkb[:, l * 8:(l + 1) * 8] = np.asarray(inp["sink_logit"][l], np.float32)[None, :]
    return dict(gains=gains, qg=qg, kvg=kvg, sinkb=sinkb, c_rope=rope, c_oh=oh, c_mult=mult, c_ident=ident)


_W_NAMES = ["w_in", "w_uq", "w_ukv", "rel_bias", "w_branch_a", "w_branch_b", "w_branch_c", "w_out",
            "ffn_w_gate", "ffn_w_up", "ffn_w_down", "router_w", "exp_w_gate", "exp_w_up", "exp_w_down"]


def kernel(**inputs):
    inp = {k: np.asarray(v) for k, v in inputs.items()}
    consts = _host_consts(inp)
    shared = {k: np.ascontiguousarray(inp[k], dtype=np.float32) for k in _W_NAMES}
    shared.update(consts)
    x = np.ascontiguousarray(inp["x"], dtype=np.float32)
    nc = build(NSEQ)
    in_maps = []
    for c in range(NCORES):
        m = dict(shared)
        m["x"] = x[c * NSEQ:(c + 1) * NSEQ]
        in_maps.append(m)
    res = run_bass_kernel_spmd(nc, in_maps, core_ids=list(range(NCORES)))
    out = np.concatenate([np.asarray(r["out"]) for r in res.results], axis=0)
    return out.astype(np.float32)
```
